# Optimizing a Trainium2 kernel written in Bass

```python
import jax, jax.numpy as jnp
from jax import lax
import numpy as np

D_MODEL = 1024
BATCH = 8
SEQ = 2048
DEPTH = 1

GRID_W = 64
POOL_WINDOWS = (2, 4, 8, 16)
POOL_WIDTH = D_MODEL // 2
POOL_GROUP = POOL_WIDTH // len(POOL_WINDOWS)
ATTN_HEADS = 8
HEAD_DIM = (D_MODEL // 2) // ATTN_HEADS
ATTN_WIDTH = ATTN_HEADS * HEAD_DIM
MIX_WIDTH = POOL_WIDTH + ATTN_WIDTH
WIN_ROWS_MAX = 8
WIN_COLS = 16
N_EXPERTS = 16
EC_CAPACITY = 2
D_EXPERT = 2 * D_MODEL
PLE_DIM = 256
RMS_EPS = 1e-6

kernel_name = "hybrid_pool_natten_ec_block"


def rms_norm(x, g):
    x32 = x.astype(jnp.float32)
    y = x32 * lax.rsqrt(jnp.mean(x32 * x32, axis=-1, keepdims=True) + RMS_EPS)
    return (y * g.astype(jnp.float32)).astype(x.dtype)


def multiscale_pool(u, w_pool, pool_scale):
    B, S, _ = u.shape
    u32 = u.astype(jnp.float32)
    cs = jnp.concatenate([jnp.zeros((B, 1, POOL_WIDTH), jnp.float32), jnp.cumsum(u32, axis=1)], axis=1)
    t = jnp.arange(S)
    outs = []
    for gi, w in enumerate(POOL_WINDOWS):
        lo = jnp.clip(t - w // 2, 0, S - 1)
        hi = jnp.clip(t + (w - w // 2) - 1, 0, S - 1)
        sl = slice(gi * POOL_GROUP, (gi + 1) * POOL_GROUP)
        csg = cs[:, :, sl]
        cnt = (hi - lo + 1).astype(jnp.float32)[None, :, None]
        outs.append((csg[:, hi + 1] - csg[:, lo]) / cnt - u32[:, :, sl])
    d = jnp.stack(outs, axis=2).astype(u.dtype)
    y = jnp.einsum('bsgc,gcd->bsgd', d, w_pool).reshape(B, S, POOL_WIDTH)
    return y * pool_scale


def neighbourhood_attention(q, k, v, q_norm, k_norm, rpb):
    B, S, H, HD = q.shape
    rows = S // GRID_W
    kh = min(WIN_ROWS_MAX, rows)
    q = rms_norm(q, q_norm) * (HD ** -0.5)
    k = rms_norm(k, k_norm)
    to_grid = lambda a: a.reshape(B, rows, GRID_W, H, HD).transpose(0, 3, 1, 2, 4)
    qg, kg, vg = to_grid(q), to_grid(k), to_grid(v)
    c = jnp.arange(GRID_W)
    col_start = jnp.clip(c - WIN_COLS // 2, 0, GRID_W - WIN_COLS)
    col_idx = col_start[:, None] + jnp.arange(WIN_COLS)[None, :]
    dc = col_idx - c[:, None] + (WIN_COLS - 1)

    def row_block(r):
        row_start = jnp.clip(r - kh // 2, 0, rows - kh)
        q_r = lax.dynamic_index_in_dim(qg, r, axis=2, keepdims=False)
        k_band = lax.dynamic_slice_in_dim(kg, row_start, kh, axis=2)
        v_band = lax.dynamic_slice_in_dim(vg, row_start, kh, axis=2)
        k_nb = k_band[:, :, :, col_idx]
        v_nb = v_band[:, :, :, col_idx]
        s = jnp.einsum('bhcd,bhicjd->bhcij', q_r, k_nb).astype(jnp.float32)
        dr = row_start + jnp.arange(kh) - r + (WIN_ROWS_MAX - 1)
        bias = rpb[:, dr[None, :, None], dc[:, None, :]]
        s = s + bias[None].astype(jnp.float32)
        pr = jax.nn.softmax(s.reshape(B, H, GRID_W, kh * WIN_COLS), axis=-1)
        pr = pr.reshape(B, H, GRID_W, kh, WIN_COLS).astype(v.dtype)
        return jnp.einsum('bhcij,bhicjd->bhcd', pr, v_nb)

    o = lax.map(row_block, jnp.arange(rows))
    return o.transpose(1, 0, 3, 2, 4).reshape(B, S, H * HD)


def expert_choice_ffn(h, w_router, w_gate, w_up, w_down):
    B, S, D = h.shape
    cap = EC_CAPACITY * S // N_EXPERTS
    aff = jax.nn.softmax(jnp.einsum('bsd,de->bse', h, w_router).astype(jnp.float32), axis=-1)
    gates, idx = lax.top_k(aff.transpose(0, 2, 1), cap)
    bidx = jnp.arange(B)[:, None, None]
    xe = h[bidx, idx]
    a = jnp.einsum('becd,edf->becf', xe, w_gate)
    b = jnp.einsum('becd,edf->becf', xe, w_up)
    ye = jnp.einsum('becf,efd->becd', jax.nn.silu(a) * b, w_down)
    return jnp.zeros_like(h).at[bidx, idx].add(ye * gates[..., None].astype(h.dtype))


def setup_inputs(seed: int = 0) -> dict:
    key = jax.random.key(seed)
    ks = jax.random.split(key, 20)
    f32 = jnp.float32
    nrm = lambda k, shape, s: jax.random.normal(k, shape, f32) * s
    gain = lambda k, shape: 1.0 + 0.02 * jax.random.normal(k, shape, f32)
    L = DEPTH
    return {
        "x": nrm(ks[0], (BATCH, SEQ, D_MODEL), 1.0),
        "p": nrm(ks[1], (L, BATCH, SEQ, PLE_DIM), 1.0),
        "norm_mix": gain(ks[2], (L, D_MODEL)),
        "w_in": nrm(ks[3], (L, D_MODEL, POOL_WIDTH + 3 * ATTN_WIDTH), D_MODEL ** -0.5),
        "w_pool": nrm(ks[4], (L, len(POOL_WINDOWS), POOL_GROUP, POOL_GROUP), POOL_GROUP ** -0.5),
        "pool_scale": gain(ks[5], (L, POOL_WIDTH)),
        "q_norm": gain(ks[6], (L, HEAD_DIM)),
        "k_norm": gain(ks[7], (L, HEAD_DIM)),
        "rpb": nrm(ks[8], (L, ATTN_HEADS, 2 * WIN_ROWS_MAX - 1, 2 * WIN_COLS - 1), 0.1),
        "w_out": nrm(ks[9], (L, MIX_WIDTH, D_MODEL), MIX_WIDTH ** -0.5),
        "norm_ffn": gain(ks[10], (L, D_MODEL)),
        "w_router": nrm(ks[11], (L, D_MODEL, N_EXPERTS), D_MODEL ** -0.5),
        "w_gate": nrm(ks[12], (L, N_EXPERTS, D_MODEL, D_EXPERT), D_MODEL ** -0.5),
        "w_up": nrm(ks[13], (L, N_EXPERTS, D_MODEL, D_EXPERT), D_MODEL ** -0.5),
        "w_down": nrm(ks[14], (L, N_EXPERTS, D_EXPERT, D_MODEL), D_EXPERT ** -0.5),
        "norm_ple": gain(ks[15], (L, D_MODEL)),
        "w_ple_gate": nrm(ks[16], (L, D_MODEL, D_MODEL), D_MODEL ** -0.5),
        "w_ple_proj": nrm(ks[17], (L, PLE_DIM, D_MODEL), PLE_DIM ** -0.5),
        "norm_ple_post": gain(ks[18], (L, D_MODEL)),
    }


def reference(x, p, norm_mix, w_in, w_pool, pool_scale, q_norm, k_norm, rpb, w_out,
              norm_ffn, w_router, w_gate, w_up, w_down, norm_ple, w_ple_gate,
              w_ple_proj, norm_ple_post):
    B, S, _ = x.shape
    for i in range(DEPTH):
        h = rms_norm(x, norm_mix[i])
        z = h @ w_in[i]
        u = z[..., :POOL_WIDTH]
        qkv = z[..., POOL_WIDTH:].reshape(B, S, 3, ATTN_HEADS, HEAD_DIM)
        y_pool = multiscale_pool(u, w_pool[i], pool_scale[i])
        y_attn = neighbourhood_attention(qkv[:, :, 0], qkv[:, :, 1], qkv[:, :, 2],
                                         q_norm[i], k_norm[i], rpb[i])
        x = x + jnp.concatenate([y_pool, y_attn], axis=-1) @ w_out[i]
        x = x + expert_choice_ffn(rms_norm(x, norm_ffn[i]), w_router[i], w_gate[i], w_up[i], w_down[i])
        g = jax.nn.sigmoid(rms_norm(x, norm_ple[i]) @ w_ple_gate[i])
        e = rms_norm(p[i] @ w_ple_proj[i], norm_ple_post[i])
        x = x + g * e
    return x
```

```python
import numpy as np
from contextlib import ExitStack
import concourse.bass as bass
import concourse.mybir as mybir
from concourse.bass_utils import run_bass_kernel_spmd

F32 = mybir.dt.float32
F32R = mybir.dt.float32r
I32 = mybir.dt.int32
ALU = mybir.AluOpType
AF = mybir.ActivationFunctionType
AX = mybir.AxisListType

S = 2048
D = 1024
NT = 16
NE = 16
CAP = 256
DFF = 2048
EPS = 1e-6
NEG = -30000.0
HSW = D + NE

QB_CHUNKS = {0: list(range(0, 6)), 1: list(range(2, 10)), 2: list(range(6, 14)), 3: list(range(10, 16))}


def bias_tile_id(qb, j):
    if qb == 0:
        return j
    if qb == 1:
        return 6 + (j - 2)
    if qb == 2:
        return 6 + (j - 6)
    return 14 + (j - 10)


def ts(i, n):
    return slice(i * n, (i + 1) * n)


class Buf:
    def __init__(self, name):
        self.name = name
        self.writes = []
        self.reads = []
        self.sem = None
        self.cnt = 0


class Op:
    __slots__ = ("eng", "fn", "deps", "dma", "sem", "val", "need_sig", "sigval")


class Prog:
    ENG = ("sync", "act", "pe", "dve", "pool")

    def __init__(self, nc, ctx):
        self.nc = nc
        self.ctx = ctx
        self.q = {k: [] for k in self.ENG}
        self.prog_sem = {k: ctx.enter_context(nc.semaphore("prog_" + k)) for k in self.ENG}
        self.nsem = 5

    def op(self, eng, fn, r=(), w=(), dma=False, sb=None, waw=False, part=False):
        o = Op()
        o.eng = eng
        o.fn = fn
        o.dma = dma
        o.need_sig = False
        o.sigval = None
        o.sem = None
        o.val = None
        raw = []
        war = []
        for b in r:
            raw.extend(b.writes)
        for b in w:
            if b.reads:
                war.extend(b.reads)
                war.extend(b.writes)
            elif not part:
                war.extend(b.writes)
        for b in w:
            if b.reads:
                b.reads = []
                b.writes = []
            b.writes.append(o)
        for b in r:
            if b not in w:
                b.reads.append(o)
        kept = []
        seen = set()
        for d in raw:
            if id(d) in seen or d is o:
                continue
            seen.add(id(d))
            if (not d.dma) and d.eng == eng and eng == "pe" and not dma:
                continue
            if not d.dma:
                d.need_sig = True
            kept.append(d)
        for d in war:
            if id(d) in seen or d is o:
                continue
            seen.add(id(d))
            if (not d.dma) and d.eng == eng and not dma:
                continue
            if not d.dma:
                d.need_sig = True
            kept.append(d)
        o.deps = kept
        if dma:
            assert sb is not None
            if sb.sem is None:
                sb.sem = self.ctx.enter_context(self.nc.semaphore("d_" + sb.name))
                self.nsem += 1
            sb.cnt += 16
            o.sem = sb.sem
            o.val = sb.cnt
        self.q[eng].append(o)
        return o

    def barrier(self, bufs):
        deps = []
        seen = set()
        for b in bufs:
            for d in list(b.writes) + list(b.reads):
                if id(d) not in seen and d.fn is not None:
                    seen.add(id(d))
                    deps.append(d)
        for k in self.ENG:
            o = Op()
            o.eng = k; o.fn = None; o.dma = False; o.need_sig = False; o.sigval = None; o.sem = None; o.val = None
            kept = []
            for d in deps:
                if (not d.dma) and d.eng == k:
                    continue
                if not d.dma:
                    d.need_sig = True
                kept.append(d)
            o.deps = kept
            self.q[k].append(o)

    def run(self):
        for k in self.ENG:
            c = 0
            for o in self.q[k]:
                if (not o.dma) and o.need_sig:
                    c += 1
                    o.sigval = c
        nc = self.nc
        engs = {"sync": None, "act": None, "pe": None, "dve": None, "pool": None}

        def emit(k, e):
            waited = {}
            for o in self.q[k]:
                need = {}
                for d in o.deps:
                    if d.dma:
                        key, v = d.sem, d.val
                    else:
                        key, v = self.prog_sem[d.eng], d.sigval
                    kk = id(key)
                    if kk not in need or need[kk][1] < v:
                        need[kk] = (key, v)
                for kk, (key, v) in need.items():
                    if waited.get(kk, 0) >= v:
                        continue
                    waited[kk] = v
                    e.wait_ge(key, v)
                if o.fn is None:
                    continue
                ins = o.fn(e)
                if o.dma:
                    ins.then_inc(o.sem, 16)
                elif o.need_sig:
                    ins.then_inc(self.prog_sem[k], 1)

        with nc.Block() as block:
            @block.sync
            def _(e):
                emit("sync", e)

            @block.scalar
            def _(e):
                emit("act", e)

            @block.tensor
            def _(e):
                emit("pe", e)

            @block.vector
            def _(e):
                emit("dve", e)

            @block.gpsimd
            def _(e):
                emit("pool", e)


class _Stop(Exception):
    pass


def build_nc(stop=99, debug=False):
    nc = bass.Bass("TRN2", target_bir_lowering=False)
    nc.dge_precook = False
    dt_in = lambda name, shape, dt=F32R: nc.dram_tensor(name, shape, dt, kind="ExternalInput").ap()
    x_d = dt_in("x", [S, D], F32)
    pT_d = dt_in("pT", [256, S])
    win_d = dt_in("w_in", [D, 2048])
    wpool_d = dt_in("w_pool", [4, 128, 128])
    wout_d = dt_in("w_out", [D, D])
    wr_d = dt_in("w_router", [D, NE])
    wg_d = dt_in("w_gate", [NE, D, DFF])
    wu_d = dt_in("w_up", [NE, D, DFF])
    wd_d = dt_in("w_down", [NE, DFF, D])
    wpg_d = dt_in("w_ple_gate", [D, D])
    wpp_d = dt_in("w_ple_proj", [256, D])
    gcols_d = dt_in("gcols", [128, 32], F32)
    gpost_d = dt_in("gpost", [128, D], F32)
    grep_d = dt_in("grep", [2, 128, D], F32)
    bias_d = dt_in("bias_tiles", [8, 20, 128, 512])
    out_d = nc.dram_tensor("out", [S, D], F32, kind="ExternalOutput").ap()
    skind = "ExternalOutput" if debug else "Internal"
    zT_d = nc.dram_tensor("zT_s", [1536, S], F32R, kind=skind).ap()
    v_d = nc.dram_tensor("v_s", [S, 512], F32R, kind=skind).ap()
    mix_d = nc.dram_tensor("mix_s", [D, S], F32R, kind=skind).ap()
    hs_d = nc.dram_tensor("hs_s", [S, HSW], F32, kind=skind).ap()
    acc_d = nc.dram_tensor("acc_s", [S, D], F32, kind=skind).ap()

    with ExitStack() as ctx:
        P = Prog(nc, ctx)
        COLS_R = 31500
        COLS_F = 19700
        bigR = ctx.enter_context(nc.sbuf_tensor("bigR", [128, COLS_R], F32R))
        bigF = ctx.enter_context(nc.sbuf_tensor("bigF", [128, COLS_F], F32))
        cur = {"R": 0, "F": 0}

        def allocR(n):
            a = cur["R"]
            cur["R"] += n
            assert cur["R"] <= COLS_R, ("R", cur["R"])
            return bigR[:, a:a + n]

        def allocF(n):
            a = cur["F"]
            cur["F"] += n
            assert cur["F"] <= COLS_F, ("F", cur["F"])
            return bigF[:, a:a + n]

        def mark():
            return dict(cur)

        def reset(m):
            cur.update(m)

        idxi_t = ctx.enter_context(nc.sbuf_tensor("idxi", [128, 4], I32))
        psb = [ctx.enter_context(nc.psum_tensor("ps%d" % i, [128, 512], F32)) for i in range(8)]
        PS = [Buf("ps%d" % i) for i in range(8)]

        ident = allocF(128)
        gcols = allocF(32)
        slotid = allocF(2)
        junkA = allocF(1024)
        junkD = allocF(512)
        B_const = Buf("const")
        B_gcols = Buf("gcols")
        B_junkA = Buf("junkA")
        B_junkD = Buf("junkD")
        P.op("sync", lambda e: e.dma_start(out=gcols, in_=gcols_d), w=[B_gcols], dma=True, sb=B_gcols)
        B_cb = Buf("cbuild")
        cb = allocF(640)
        aff_all = allocF(256)
        P.op("pool", lambda e: e.memset(ident, 1.0), w=[B_cb, B_const])
        P.op("pool", lambda e: e.affine_select(out=ident, in_=ident, pattern=[[-1, 128]], compare_op=ALU.is_equal,
                                               fill=0.0, base=0, channel_multiplier=1), w=[B_cb, B_const])
        P.op("pool", lambda e: e.iota(slotid, pattern=[[128, 2]], base=0, channel_multiplier=1,
                                      allow_small_or_imprecise_dtypes=True), w=[B_cb, B_const])
        P.op("pool", lambda e: e.memset(cb, 0.0), w=[B_cb])
        P.op("pool", lambda e: e.tensor_copy(out=cb[:, 0:128], in_=ident), w=[B_cb])
        P.op("pool", lambda e: e.memset(cb[0:64, 128:192], 1.0 / 64), w=[B_cb])
        P.op("pool", lambda e: e.memset(cb[64:128, 192:256], 1.0 / 64), w=[B_cb])
        P.op("pool", lambda e: e.memset(cb[:, 256:320], 1.0), w=[B_cb])
        P.op("pool", lambda e: e.memset(cb[:, 448:512], 1.0), w=[B_cb])
        P.op("pool", lambda e: e.memset(cb[:, 512:640], 1.0), w=[B_cb])
        crr = allocR(640)
        onesall = crr[:, 512:640]
        identr = crr[:, 0:128]
        blk64 = crr[:, 128:256]
        onesA = crr[:, 256:384]
        onesB = crr[:, 384:512]
        P.op("dve", lambda e: e.tensor_copy(out=crr.bitcast(F32R), in_=cb), r=[B_cb], w=[B_const])
        B_zT = Buf("zT"); B_v = Buf("v_d"); B_mix = Buf("mix_d"); B_acc = Buf("acc_d"); B_hs = Buf("hs_d"); B_out = Buf("out")
        DRAMB = [B_zT, B_v, B_mix, B_acc, B_hs, B_out]

        def finish():
            P.op("pool", None, r=DRAMB)
            P.op("sync", None, r=DRAMB)
            P.run()

        base_mark = mark()

        def rstd_from_ss(ss_col, out_col, bufs_r, bufs_w, n):
            P.op("act", lambda e: e.activation(out=out_col, in_=ss_col, func=AF.Ln, bias=EPS, scale=1.0 / n), r=bufs_r, w=bufs_w)
            P.op("act", lambda e: e.activation(out=out_col, in_=out_col, func=AF.Exp, scale=-0.5), r=bufs_w, w=bufs_w)

        evac_flip = [0]

        def evac_scale(out_ap, in_ap, scal_ap, r, w):
            evac_flip[0] ^= 1
            if evac_flip[0]:
                return P.op("act", lambda e: e.activation(out=out_ap, in_=in_ap, func=AF.Copy, scale=scal_ap), r=r, w=w, part=True)
            return P.op("dve", lambda e: e.tensor_scalar(out=out_ap, in0=in_ap, scalar1=scal_ap, scalar2=None, op0=ALU.mult), r=r, w=w, part=True)

        def evac_copy(out_ap, in_ap, r, w):
            evac_flip[0] ^= 1
            if evac_flip[0]:
                return P.op("act", lambda e: e.activation(out=out_ap, in_=in_ap, func=AF.Copy), r=r, w=w, part=True)
            return P.op("dve", lambda e: e.tensor_copy(out=out_ap, in_=in_ap), r=r, w=w, part=True)

        win = allocR(8 * 2048)
        win3 = win.rearrange("p (c n) -> p c n", c=8)
        B_win = [Buf("win%d" % i) for i in range(8)]
        xc = [allocF(1024) for _ in range(8)]
        B_xc = [Buf("xc%d" % i) for i in range(8)]
        stat = allocF(64)
        B_stat = [Buf("stat%d" % i) for i in range(16)]
        P.op("pool", lambda e: e.memset(stat, 0.0), w=B_stat)
        hTb = [allocR(8 * 512) for _ in range(2)]
        B_hT = [[Buf("hT%d_%d" % (s_, dc)) for dc in range(8)] for s_ in range(2)]
        stg = [allocR(512) for _ in range(4)]
        B_stg = [Buf("stg%d" % i) for i in range(4)]
        stg_i = [0]
        ps_i = [0]

        def next_ps(lo, hi):
            i = lo + (ps_i[0] % (hi - lo))
            ps_i[0] += 1
            return i

        def store(out_dram, stg_ap, stg_buf, bdram, eng="pool"):
            P.op(eng, lambda e: e.dma_start(out=out_dram, in_=stg_ap.bitcast(F32R)), r=[stg_buf], w=[bdram],
                 dma=True, sb=stg_buf)

        def s1_front_chunk(tb, j):
            xo = (tb % 2) * 4
            c = tb * 4 + j
            xj = xc[xo + j]
            P.op("sync", lambda e, c=c, xj=xj: e.dma_start(out=xj, in_=x_d[ts(c, 128), :]), w=[B_xc[xo + j]], dma=True, sb=B_xc[xo + j])
            P.op("act", lambda e, c=c, xj=xj: e.activation(out=junkA, in_=xj, func=AF.Square, accum_out=stat[:, c:c + 1]),
                 r=[B_xc[xo + j]], w=[B_junkA, B_stat[c]])
            rstd_from_ss(stat[:, c:c + 1], stat[:, 16 + c:17 + c], [B_stat[c]], [B_stat[c]], D)
            P.op("act", lambda e, c=c, xj=xj: e.activation(out=xj, in_=xj, func=AF.Copy, scale=stat[:, 16 + c:17 + c]),
                 r=[B_xc[xo + j], B_stat[c]], w=[B_xc[xo + j]])

        def s1_front(tb):
            for j in range(4):
                s1_front_chunk(tb, j)

        def s1_back(tb):
            hs_ = tb % 2
            hT3 = hTb[hs_].rearrange("p (c n) -> p c n", c=8)
            xo = (tb % 2) * 4
            for dc in range(8):
                pi = next_ps(0, 2)
                for j in range(4):
                    P.op("pe", lambda e, pi=pi, j=j, dc=dc, xj=xc[xo + j]: e.transpose(out=psb[pi][:, ts(j, 128)], in_=xj[:, ts(dc, 128)], identity=ident),
                         r=[B_xc[xo + j], B_const], w=[PS[pi]])
                evac_scale(hT3[:, dc, :].bitcast(F32R), psb[pi][:, :], gcols[:, dc:dc + 1], [PS[pi], B_gcols], [B_hT[hs_][dc]])
            for n in range(12):
                pi = next_ps(2, 8)
                for dc in range(8):
                    P.op("pe", lambda e, pi=pi, n=n, dc=dc, hT3=hT3: e.matmul(psb[pi][:, :], lhsT=win3[:, dc, ts(n, 128)].bitcast(F32R),
                                                                          rhs=hT3[:, dc, :].bitcast(F32R), start=(dc == 0), stop=(dc == 7)),
                         r=[B_win[dc], B_hT[hs_][dc]], w=[PS[pi]])
                si = stg_i[0] % 4
                stg_i[0] += 1
                evac_copy(stg[si].bitcast(F32R), psb[pi][:, :], [PS[pi]], [B_stg[si]])
                store(zT_d[ts(n, 128), ts(tb, 512)], stg[si], B_stg[si], B_zT)
                if tb + 1 < 4 and n % 3 == 2:
                    s1_front_chunk(tb + 1, n // 3)
            for j in range(4):
                c = tb * 4 + j
                pi = next_ps(2, 8)
                for dc in range(8):
                    P.op("pe", lambda e, pi=pi, j=j, dc=dc, hT3=hT3: e.matmul(psb[pi][:, :], lhsT=hT3[:, dc, ts(j, 128)].bitcast(F32R),
                                                                          rhs=win3[:, dc, 1536:2048].bitcast(F32R), start=(dc == 0), stop=(dc == 7)),
                         r=[B_win[dc], B_hT[hs_][dc]], w=[PS[pi]])
                si = stg_i[0] % 4
                stg_i[0] += 1
                evac_copy(stg[si].bitcast(F32R), psb[pi][:, :], [PS[pi]], [B_stg[si]])
                store(v_d[ts(c, 128), :], stg[si], B_stg[si], B_v)

        s1_front(0)
        for dc in range(8):
            P.op("sync", lambda e, dc=dc: e.dma_start(out=win3[:, dc, :].bitcast(F32R), in_=win_d[ts(dc, 128), :]),
                 w=[B_win[dc]], dma=True, sb=B_win[dc])
        for tb in range(4):
            s1_back(tb)

        if stop == 1:
            finish()
            return nc
        reset(base_mark)
        stg = [allocR(512) for _ in range(4)]
        B_region = Buf("region")

        def region_barrier(bufs):
            P.barrier(bufs)

        all_s1 = B_win + [B_zT, B_v] + B_xc + B_stat + B_hT[0] + B_hT[1] + B_stg + PS + [B_junkA]
        region_barrier(all_s1)
        B_stg = [Buf("stg2_%d" % i) for i in range(4)]
        upad = [allocF(2064) for _ in range(2)]
        B_up = [Buf("upad%d" % i) for i in range(2)]
        ta = allocF(2064)
        tb_ = allocF(2064)
        rc = allocF(2048)
        dn = allocR(2048)
        wpool = allocR(4 * 128)
        wpool3 = wpool.rearrange("p (g n) -> p g n", g=4)
        B_ta = Buf("ta")
        B_tb = Buf("tb")
        B_rc = Buf("rc")
        B_dn = Buf("dn")
        B_wpool = Buf("wpool")
        P.op("sync", lambda e: e.dma_start(out=wpool3.bitcast(F32R), in_=wpool_d.rearrange("g c n -> c g n")), w=[B_wpool], dma=True, sb=B_wpool)
        for i in range(2):
            P.op("pool", lambda e, i=i: e.memset(upad[i], 0.0), w=[B_up[i]])
        def emit_pool_dve(g):
            wsz = (2, 4, 8, 16)[g]
            u = upad[g % 2]
            bu = B_up[g % 2]
            P.op("sync", lambda e, g=g, u=u: e.dma_start(out=u[:, 8:2056].bitcast(F32R), in_=zT_d[ts(g, 128), :]), r=[B_zT], w=[bu], dma=True, sb=bu)
            hw = wsz // 2
            P.op("pool", lambda e, wsz=wsz: e.memset(rc, 1.0 / wsz), w=[B_rc])
            for t in range(hw):
                P.op("pool", lambda e, t=t, hw=hw: e.memset(rc[:, t:t + 1], 1.0 / (t + hw)), w=[B_rc])
            for t in range(S - hw + 1, S):
                P.op("pool", lambda e, t=t, hw=hw: e.memset(rc[:, t:t + 1], 1.0 / (S - t + hw)), w=[B_rc])
            P.op("pool", lambda e, u=u: e.tensor_tensor(out=ta[:, 0:2063], in0=u[:, 0:2063], in1=u[:, 1:2064], op=ALU.add), r=[bu], w=[B_ta])
            src, bsrc, width = ta, B_ta, 2063
            other, bother = tb_, B_tb
            step = 2
            while step < wsz:
                nw = width - step
                P.op("pool", lambda e, src=src, other=other, nw=nw, step=step: e.tensor_tensor(out=other[:, 0:nw], in0=src[:, 0:nw], in1=src[:, step:step + nw], op=ALU.add),
                     r=[bsrc], w=[bother])
                src, bsrc, other, bother = other, bother, src, bsrc
                width = nw
                step *= 2
            off = 8 - hw
            P.op("pool", lambda e, src=src, other=other, off=off: e.tensor_tensor(out=other[:, 0:2048], in0=src[:, off:off + 2048], in1=rc, op=ALU.mult),
                 r=[bsrc, B_rc], w=[bother])
            P.op("pool", lambda e, other=other, u=u: e.tensor_tensor(out=dn.bitcast(F32R), in0=other[:, 0:2048], in1=u[:, 8:2056], op=ALU.subtract),
                 r=[bother, bu], w=[B_dn])

        def emit_pool_mm(g):
            for tb in range(4):
                pi = 7
                P.op("pe", lambda e, pi=pi, g=g, tb=tb: e.matmul(psb[pi][:, :], lhsT=wpool3[:, g, :].bitcast(F32R), rhs=dn[:, ts(tb, 512)].bitcast(F32R), start=True, stop=True),
                     r=[B_wpool, B_dn], w=[PS[pi]])
                si = stg_i[0] % 4
                stg_i[0] += 1
                evac_scale(stg[si].bitcast(F32R), psb[pi][:, :], gcols[:, 24 + g:25 + g], [PS[pi], B_gcols], [B_stg[si]])
                store(mix_d[ts(g, 128), ts(tb, 512)], stg[si], B_stg[si], B_mix)

        if stop == 2:
            for g in range(4):
                emit_pool_dve(g)
                emit_pool_mm(g)
            finish()
            return nc
        qraw = [allocF(2048)] * 2
        kraw = [allocF(2048)] * 2
        B_qraw = [Buf("qraw")] * 2
        B_kraw = [Buf("kraw")] * 2
        sq = allocR(2048)
        B_sq = Buf("sq")
        rsd = allocF(512)
        B_rsd = Buf("rsd")
        qn = [allocR(2048) for _ in range(2)]
        knA = [allocR(2048) for _ in range(2)]
        knB = [allocR(2048) for _ in range(2)]
        B_qn = [Buf("qn%d" % i) for i in range(2)]
        B_kn = [Buf("kn%d" % i) for i in range(2)]
        vAl = [allocR(16 * 128) for _ in range(2)]
        vBl = [allocR(16 * 128) for _ in range(2)]
        vA3l = [t.rearrange("p (c n) -> p c n", c=16) for t in vAl]
        vB3l = [t.rearrange("p (c n) -> p c n", c=16) for t in vBl]
        B_vAl = [Buf("vA%d" % i) for i in range(2)]
        B_vBl = [Buf("vB%d" % i) for i in range(2)]
        bt = [allocR(512) for _ in range(4)]
        B_bt = [Buf("bt%d" % i) for i in range(4)]
        Pt = [allocR(512) for _ in range(3)]
        B_Pt = [Buf("Pt%d" % i) for i in range(3)]
        rD = allocF(512)
        B_rD = Buf("rD")
        tmpS = [allocF(512) for _ in range(3)]
        B_tmpS = [Buf("tmpS%d" % i) for i in range(3)]
        for q4 in range(4):
            for i2 in range(2):
                P.op("dve", lambda e, q4=q4, i2=i2: e.tensor_scalar(out=knA[i2][64:128, ts(q4, 512)], in0=cb[64:128, 0:512], scalar1=0.0, scalar2=None, op0=ALU.mult), r=[B_cb], w=[B_kn[i2]])
                P.op("dve", lambda e, q4=q4, i2=i2: e.tensor_scalar(out=knB[i2][0:64, ts(q4, 512)], in0=cb[0:64, 0:512], scalar1=0.0, scalar2=None, op0=ALU.mult), r=[B_cb], w=[B_kn[i2]])
        for q4 in range(4):
            for i2 in range(2):
                P.op("dve", lambda e, q4=q4, i2=i2: e.tensor_scalar(out=vAl[i2][:, ts(q4, 512)], in0=cb[:, 0:512], scalar1=0.0, scalar2=None, op0=ALU.mult), r=[B_cb], w=[B_vAl[i2]])
                P.op("dve", lambda e, q4=q4, i2=i2: e.tensor_scalar(out=vBl[i2][:, ts(q4, 512)], in0=cb[:, 0:512], scalar1=0.0, scalar2=None, op0=ALU.mult), r=[B_cb], w=[B_vBl[i2]])
        v_d3 = v_d.rearrange("(c p) n -> p c n", p=128)
        bt_i = [0]
        pt_i = [0]
        def setup_pieces(pr):
            sl = pr % 2
            pieces = []

            def loads():
                P.op("sync", lambda e, pr=pr, sl=sl: e.dma_start(out=qraw[sl].bitcast(F32R), in_=zT_d[512 + pr * 128:512 + (pr + 1) * 128, :]), r=[B_zT], w=[B_qraw[sl]], dma=True, sb=B_qraw[sl])
                P.op("sync", lambda e, pr=pr, sl=sl: e.dma_start(out=kraw[sl].bitcast(F32R), in_=zT_d[1024 + pr * 128:1024 + (pr + 1) * 128, :]), r=[B_zT], w=[B_kraw[sl]], dma=True, sb=B_kraw[sl])
                P.op("sync", lambda e, pr=pr, sl=sl: e.dma_start(out=vA3l[sl][:, :, 0:64].bitcast(F32R), in_=v_d3[:, :, pr * 128:pr * 128 + 64]), r=[B_v], w=[B_vAl[sl]], dma=True, sb=B_vAl[sl])
                P.op("sync", lambda e, pr=pr, sl=sl: e.dma_start(out=vB3l[sl][:, :, 64:128].bitcast(F32R), in_=v_d3[:, :, pr * 128 + 64:pr * 128 + 128]), r=[B_v], w=[B_vBl[sl]], dma=True, sb=B_vBl[sl])
            pieces.append(loads)
            for which in range(2):
                raw = (qraw, kraw)[which][sl]
                braw = (B_qraw, B_kraw)[which][sl]
                bdst = (B_qn, B_kn)[which][sl]

                def square(raw=raw, braw=braw):
                    P.op("act", lambda e, raw=raw: e.activation(out=sq.bitcast(F32R), in_=raw, func=AF.Square), r=[braw], w=[B_sq])
                pieces.append(square)
                for tb in range(4):
                    def norm_tb(which=which, raw=raw, braw=braw, bdst=bdst, tb=tb):
                        pi = 7
                        P.op("pe", lambda e, pi=pi, tb=tb: e.matmul(psb[pi][:, :], lhsT=blk64.bitcast(F32R), rhs=sq[:, ts(tb, 512)].bitcast(F32R), start=True, stop=True),
                             r=[B_const, B_sq], w=[PS[pi]])
                        sc_ = 64.0 if which == 0 else 1.0
                        P.op("act", lambda e, pi=pi, sc_=sc_: e.activation(out=rsd, in_=psb[pi][:, :], func=AF.Ln, bias=EPS * sc_, scale=sc_), r=[PS[pi]], w=[B_rsd])
                        P.op("act", lambda e: e.activation(out=rsd, in_=rsd, func=AF.Exp, scale=-0.5), r=[B_rsd], w=[B_rsd])
                        if which == 0:
                            P.op("dve", lambda e, raw=raw, tb=tb: e.scalar_tensor_tensor(out=qn[sl][:, ts(tb, 512)].bitcast(F32R), in0=raw[:, ts(tb, 512)], scalar=gcols[:, 28:29],
                                                                                     in1=rsd, op0=ALU.mult, op1=ALU.mult),
                                 r=[braw, B_rsd, B_gcols], w=[bdst])
                        else:
                            for (lo, hi, kdst) in ((0, 64, knA[sl]), (64, 128, knB[sl])):
                                P.op("dve", lambda e, raw=raw, kdst=kdst, tb=tb, lo=lo, hi=hi: e.scalar_tensor_tensor(out=kdst[lo:hi, ts(tb, 512)].bitcast(F32R), in0=raw[lo:hi, ts(tb, 512)],
                                                                                                              scalar=gcols[lo:hi, 29:30], in1=rsd[lo:hi, :], op0=ALU.mult, op1=ALU.mult),
                                     r=[braw, B_rsd, B_gcols], w=[bdst])
                    pieces.append(norm_tb)
            return pieces

        for f_ in setup_pieces(0):
            f_()
        emit_pool_dve(0)
        for pr in range(4):
            sl = pr % 2
            vA3 = vA3l[sl]
            vB3 = vB3l[sl]
            B_vA = B_vAl[sl]
            B_vB = B_vBl[sl]
            nxt = setup_pieces(pr + 1) if pr + 1 < 4 else []
            sched = {}
            slots_ = [2, 16, 18, 20, 22, 24, 26, 28, 30, 32, 34]
            for k_, f_ in enumerate(nxt):
                sched.setdefault(slots_[k_], []).append(f_)
            sched.setdefault(42, []).append(lambda pr=pr: emit_pool_mm(pr))
            if pr + 1 < 4:
                sched.setdefault(46, []).append(lambda pr=pr: emit_pool_dve(pr + 1))
            steps = [(qb, hh, j) for qb in range(4) for hh in range(2) for j in QB_CHUNKS[qb]]
            nst = len(steps)
            bslot = {}
            qk_info = {}

            def emit_bdma(si_):
                qb, hh, j = steps[si_]
                h = pr * 2 + hh
                bi = bt_i[0] % 4
                bt_i[0] += 1
                bslot[si_] = bi
                P.op("sync", lambda e, h=h, qb=qb, j=j, bi=bi: e.dma_start(out=bt[bi].bitcast(F32R), in_=bias_d[h, bias_tile_id(qb, j), :, :]), w=[B_bt[bi]], dma=True, sb=B_bt[bi])

            def emit_qk(si_):
                qb, hh, j = steps[si_]
                pi = next_ps(0, 3)
                bi = bslot[si_]
                hb = 64 * hh
                kpad = knA[sl] if hh == 0 else knB[sl]
                P.op("pe", lambda e, pi=pi, kpad=kpad, j=j, qb=qb, sl=sl: e.matmul(psb[pi][:, :], lhsT=kpad[:, ts(j, 128)].bitcast(F32R),
                                                                             rhs=qn[sl][:, ts(qb, 512)].bitcast(F32R), start=True, stop=True),
                     r=[B_kn[sl], B_qn[sl]], w=[PS[pi]])
                ti = pt_i[0] % 3
                pt_i[0] += 1
                P.op("dve", lambda e, pi=pi, bi=bi, ti=ti: e.tensor_tensor(out=tmpS[ti], in0=psb[pi][:, :], in1=bt[bi].bitcast(F32), op=ALU.add),
                     r=[PS[pi], B_bt[bi]], w=[B_tmpS[ti]])
                P.op("act", lambda e, ti=ti: e.activation(out=Pt[ti].bitcast(F32R), in_=tmpS[ti], func=AF.Exp), r=[B_tmpS[ti]], w=[B_Pt[ti]])
                qk_info[si_] = ti

            def emit_pv(si_):
                qb, hh, j = steps[si_]
                nps, dps = (3, 4) if (qb % 2 == 0) else (5, 6)
                ti = qk_info[si_]
                first = (si_ == 0) or (steps[si_ - 1][0] != qb)
                last = (si_ == nst - 1) or (steps[si_ + 1][0] != qb)
                vt3 = vA3 if hh == 0 else vB3
                bv = B_vA if hh == 0 else B_vB
                on = onesA if hh == 0 else onesB
                extra = []
                P.op("pe", lambda e, vt3=vt3, j=j, ti=ti, first=first, last=last, nps=nps: e.matmul(psb[nps][:, :], lhsT=vt3[:, j, :].bitcast(F32R), rhs=Pt[ti].bitcast(F32R), start=first, stop=last),
                     r=[bv, B_Pt[ti]] + extra, w=[PS[nps]])
                P.op("pe", lambda e, on=on, ti=ti, first=first, last=last, dps=dps: e.matmul(psb[dps][:, :], lhsT=on.bitcast(F32R), rhs=Pt[ti].bitcast(F32R), start=first, stop=last),
                     r=[B_const, B_Pt[ti]], w=[PS[dps]])
                if last:
                    P.op("act", lambda e, dps=dps: e.activation(out=rD, in_=psb[dps][:, :], func=AF.Ln), r=[PS[dps]], w=[B_rD])
                    P.op("act", lambda e: e.activation(out=rD, in_=rD, func=AF.Exp, scale=-1.0), r=[B_rD], w=[B_rD])
                    si = stg_i[0] % 4
                    stg_i[0] += 1
                    P.op("dve", lambda e, nps=nps, si=si: e.tensor_tensor(out=stg[si].bitcast(F32R), in0=psb[nps][:, :], in1=rD, op=ALU.mult), r=[PS[nps], B_rD], w=[B_stg[si]])
                    store(mix_d[512 + pr * 128:512 + (pr + 1) * 128, ts(qb, 512)], stg[si], B_stg[si], B_mix)

            for si_ in range(min(3, nst)):
                emit_bdma(si_)
            emit_qk(0)
            if nst > 1:
                emit_qk(1)
            for si_ in range(nst):
                if si_ + 3 < nst:
                    emit_bdma(si_ + 3)
                if si_ + 2 < nst:
                    emit_qk(si_ + 2)
                emit_pv(si_)
                for f_ in sched.get(si_, []):
                    f_()

        if stop == 3:
            finish()
            return nc
        all_s3 = (B_tmpS + B_vAl + B_vBl + [B_zT, B_v, B_mix, B_sq, B_rsd, B_rD, B_ta, B_tb, B_rc, B_dn, B_wpool] + B_up + B_qraw + B_kraw + B_qn + B_kn + B_bt + B_Pt + B_stg + PS)
        region_barrier(all_s3)
        reset(base_mark)
        wo = allocR(8 * 1024)
        wo3 = wo.rearrange("p (c n) -> p c n", c=8)
        B_wo = Buf("wo")
        P.op("sync", lambda e: e.dma_start(out=wo3.bitcast(F32R), in_=wout_d.rearrange("(c p) n -> p c n", p=128)), w=[B_wo], dma=True, sb=B_wo)
        wr = allocR(8 * 16)
        wr3 = wr.rearrange("p (c n) -> p c n", c=8)
        B_wr = Buf("wr")
        P.op("sync", lambda e: e.dma_start(out=wr3.bitcast(F32R), in_=wr_d.rearrange("(c p) n -> p c n", p=128)), w=[B_wr], dma=True, sb=B_wr)
        gffn_rep = allocF(1024)
        B_gfr = Buf("gffn_rep")
        P.op("sync", lambda e: e.dma_start(out=gffn_rep, in_=grep_d[0]), w=[B_gfr], dma=True, sb=B_gfr)
        mT = [allocR(8 * 128) for _ in range(2)]
        B_mT = [Buf("mT%d" % i) for i in range(2)]
        xc = [allocF(1024) for _ in range(2)]
        B_xc = [Buf("xc4_%d" % i) for i in range(2)]
        x1c = [allocF(1024) for _ in range(2)]
        B_x1 = [Buf("x1c%d" % i) for i in range(2)]
        xs2 = [allocF(HSW) for _ in range(3)]
        B_xs2 = [Buf("xs2_%d" % i) for i in range(3)]
        h2T = [allocR(8 * 128) for _ in range(2)]
        B_h2T = [Buf("h2T%d" % i) for i in range(2)]
        aff3 = aff_all.rearrange("p (c n) -> p c n", c=16)
        B_aff = [Buf("aff%d" % i) for i in range(16)]
        st4 = allocF(64)
        B_st4 = [Buf("st4_%d" % i) for i in range(16)]
        P.op("pool", lambda e: e.memset(st4, 0.0), w=B_st4)
        mix_d3 = mix_d.rearrange("(c p) t -> p c t", p=128)
        def s4_front(c):
            s2 = c % 2
            mT3 = mT[s2].rearrange("p (c n) -> p c n", c=8)
            P.op("sync", lambda e, c=c, mT3=mT3: e.dma_start(out=mT3.bitcast(F32R), in_=mix_d3[:, :, ts(c, 128)]), r=[B_mix], w=[B_mT[s2]], dma=True, sb=B_mT[s2])
            P.op("sync", lambda e, c=c, s2=s2: e.dma_start(out=xc[s2], in_=x_d[ts(c, 128), :]), w=[B_xc[s2]], dma=True, sb=B_xc[s2])
            for half in range(2):
                pi = half
                for k in range(8):
                    P.op("pe", lambda e, pi=pi, k=k, half=half, mT3=mT3: e.matmul(psb[pi][:, :], lhsT=mT3[:, k, :].bitcast(F32R), rhs=wo3[:, k, ts(half, 512)].bitcast(F32R),
                                                                            start=(k == 0), stop=(k == 7)),
                         r=[B_mT[s2], B_wo], w=[PS[pi]])
                P.op("dve", lambda e, pi=pi, half=half, s2=s2: e.tensor_tensor(out=x1c[s2][:, ts(half, 512)], in0=psb[pi][:, :], in1=xc[s2][:, ts(half, 512)], op=ALU.add),
                     r=[PS[pi], B_xc[s2]], w=[B_x1[s2]])
            P.op("pool", lambda e, c=c, s2=s2: e.dma_start(out=acc_d[ts(c, 128), :], in_=x1c[s2]), r=[B_x1[s2]], w=[B_acc], dma=True, sb=B_x1[s2])
            P.op("act", lambda e, c=c, s2=s2: e.activation(out=junkA, in_=x1c[s2], func=AF.Square, accum_out=st4[:, c:c + 1]), r=[B_x1[s2]], w=[B_junkA, B_st4[c]])
            rstd_from_ss(st4[:, c:c + 1], st4[:, 16 + c:17 + c], [B_st4[c]], [B_st4[c]], D)
            P.op("dve", lambda e, c=c, s2=s2: e.scalar_tensor_tensor(out=xs2[c % 3][:, 0:D], in0=x1c[s2], scalar=st4[:, 16 + c:17 + c], in1=gffn_rep, op0=ALU.mult, op1=ALU.mult),
                 r=[B_x1[s2], B_st4[c], B_gfr], w=[B_xs2[c % 3]])
            pA = 4 + 2 * s2
            pB = 5 + 2 * s2
            for dc in range(8):
                pi = pA if dc < 4 else pB
                P.op("pe", lambda e, pi=pi, dc=dc, s2=s2: e.transpose(out=psb[pi][:, ts(dc % 4, 128)], in_=xs2[c % 3][:, ts(dc, 128)], identity=ident),
                     r=[B_xs2[c % 3], B_const], w=[PS[pi]])
            evac_copy(h2T[s2][:, 0:512], psb[pA][:, :], [PS[pA]], [B_h2T[s2]])
            evac_copy(h2T[s2][:, 512:1024], psb[pB][:, :], [PS[pB]], [B_h2T[s2]])

        def s4_back(c):
            s2 = c % 2
            h2T3 = h2T[s2].rearrange("p (c n) -> p c n", c=8)
            pr_ = 2 + s2
            for dc in range(8):
                P.op("pe", lambda e, pr_=pr_, dc=dc, h2T3=h2T3: e.matmul(psb[pr_][:, 0:16], lhsT=h2T3[:, dc, :].bitcast(F32R), rhs=wr3[:, dc, :].bitcast(F32R), start=(dc == 0), stop=(dc == 7)),
                     r=[B_h2T[s2], B_wr], w=[PS[pr_]])
            P.op("dve", lambda e, pr_=pr_, c=c: e.tensor_reduce(out=st4[:, 32 + c:33 + c], in_=psb[pr_][:, 0:16], axis=AX.X, op=ALU.max), r=[PS[pr_]], w=[B_st4[c]])
            P.op("dve", lambda e, c=c: e.tensor_scalar(out=st4[:, 32 + c:33 + c], in0=st4[:, 32 + c:33 + c], scalar1=-1.0, scalar2=None, op0=ALU.mult), r=[B_st4[c]], w=[B_st4[c]])
            P.op("act", lambda e, pr_=pr_, c=c: e.activation(out=aff3[:, c, :], in_=psb[pr_][:, 0:16], func=AF.Exp, bias=st4[:, 32 + c:33 + c], scale=1.0, accum_out=st4[:, 48 + c:49 + c]),
                 r=[PS[pr_], B_st4[c]], w=[B_aff[c], B_st4[c]])
            P.op("dve", lambda e, c=c: e.reciprocal(out=st4[:, 48 + c:49 + c], in_=st4[:, 48 + c:49 + c]), r=[B_st4[c]], w=[B_st4[c]])
            P.op("dve", lambda e, c=c: e.tensor_scalar(out=aff3[:, c, :], in0=aff3[:, c, :], scalar1=st4[:, 48 + c:49 + c], scalar2=None, op0=ALU.mult), r=[B_st4[c], B_aff[c]], w=[B_aff[c]])
            P.op("dve", lambda e, c=c, s2=s2: e.tensor_copy(out=xs2[c % 3][:, D:HSW], in_=aff3[:, c, :]), r=[B_aff[c]], w=[B_xs2[c % 3]])
            P.op("pool", lambda e, c=c, s2=s2: e.dma_start(out=hs_d[ts(c, 128), :], in_=xs2[c % 3]), r=[B_xs2[c % 3]], w=[B_hs], dma=True, sb=B_xs2[c % 3])

        s4_front(0)
        for c in range(NT):
            if c + 1 < NT:
                s4_front(c + 1)
            s4_back(c)

        if stop == 4:
            finish()
            return nc
        all_s4 = [B_wo, B_wr, B_mix, B_junkA] + B_mT + B_xc + B_x1 + B_xs2 + B_h2T + B_st4 + PS
        region_barrier(all_s4)
        reset(base_mark)
        xe_all = allocF(2 * 2 * HSW)
        affT = xe_all[:, 0:2048]
        work = xe_all[:, 2048:4096]
        cum = allocR(2048)
        cume = allocR(2048)
        B_cume = Buf("cume")
        mx8 = allocF(8)
        B_affT = Buf("affT")
        B_work = Buf("work")
        B_cum = Buf("cum")
        for tb in range(4):
            pi = next_ps(0, 4)
            for j in range(4):
                c = tb * 4 + j
                P.op("pe", lambda e, pi=pi, j=j, c=c: e.matmul(psb[pi][0:16, ts(j, 128)], lhsT=aff3[:, c, :], rhs=ident, start=True, stop=True),
                     r=[B_aff[c], B_const], w=[PS[pi]])
            P.op("dve", lambda e, pi=pi, tb=tb: e.tensor_copy(out=affT[0:16, ts(tb, 512)], in_=psb[pi][0:16, :]), r=[PS[pi]], w=[B_affT])
        for rnd in range(CAP // 8):
            srcw = affT if rnd == 0 else work
            P.op("dve", lambda e, srcw=srcw: e.max(out=mx8[0:16, :], in_=srcw[0:16, :]), r=[B_affT, B_work], w=[B_work])
            if rnd < CAP // 8 - 1:
                P.op("dve", lambda e, srcw=srcw: e.match_replace(out=work[0:16, :], in_to_replace=mx8[0:16, :], in_values=srcw[0:16, :], imm_value=-1.0),
                     r=[B_affT, B_work], w=[B_work])
        P.op("dve", lambda e: e.tensor_scalar(out=work[0:16, :], in0=affT[0:16, :], scalar1=mx8[0:16, 7:8], scalar2=None, op0=ALU.is_ge), r=[B_affT, B_work], w=[B_work])
        P.op("dve", lambda e: e.tensor_tensor_scan(out=cum[0:16, :].bitcast(F32R), data0=work[0:16, :], data1=work[0:16, :], initial=0.0, op0=ALU.add, op1=ALU.max),
             r=[B_work], w=[B_cum])

        moe_mark = mark()
        NW = 5
        wring = [allocR(4096) for _ in range(NW)]
        B_wr_ = [Buf("wring%d" % i) for i in range(NW)]
        xe = [xe_all[:, 0:2 * HSW], xe_all[:, 2 * HSW:4 * HSW]]
        B_xe = [Buf("xe%d" % i) for i in range(2)]
        xeT = [allocR(8 * 256)] * 2
        B_xeT = [Buf("xeT")] * 2
        actb = allocR(16 * 256)
        act3 = actb.rearrange("p (c n) -> p c n", c=16)
        B_act = [Buf("act%d" % i) for i in range(16)]
        sg = [allocF(256) for _ in range(2)]
        B_sg = [Buf("sg%d" % i) for i in range(2)]
        ye = [allocF(2 * 1024) for _ in range(2)]
        B_ye = [Buf("ye%d" % i) for i in range(2)]
        cnt = allocF(16)
        idxf = allocF(4)
        B_idx = [Buf("idx%d" % i) for i in range(2)]
        B_cnt = Buf("cnt")
        wi = [0]

        def wload(dram_ap, shape_c):
            i = wi[0] % NW
            wi[0] += 1
            view = wring[i].rearrange("p (c n) -> p c n", c=shape_c)
            P.op("sync", lambda e, view=view, dram_ap=dram_ap: e.dma_start(out=view.bitcast(F32R), in_=dram_ap), w=[B_wr_[i]], dma=True, sb=B_wr_[i])
            return view, B_wr_[i]

        def emit_idx(ex):
            s_ = ex % 2
            P.op("dve", lambda e, ex=ex: e.tensor_scalar(out=cume[0:16, :], in0=cum[0:16, :].bitcast(F32), scalar1=ident[0:16, ex:ex + 1], scalar2=None, op0=ALU.mult),
                 r=[B_cum, B_const], w=[B_cume])
            for tb in range(4):
                pi = next_ps(0, 2)
                P.op("pe", lambda e, pi=pi, tb=tb: e.matmul(psb[pi][:, :], lhsT=onesall[0:16, :], rhs=cume[0:16, ts(tb, 512)], start=True, stop=True),
                     r=[B_const, B_cume], w=[PS[pi]])
                for sc in range(2):
                    P.op("dve", lambda e, pi=pi, tb=tb, sc=sc: e.tensor_scalar(out=junkD, in0=psb[pi][:, :], scalar1=slotid[:, sc:sc + 1], scalar2=0.0, op0=ALU.is_le, op1=ALU.add,
                                                                         accum_out=cnt[:, sc * 4 + tb:sc * 4 + tb + 1]),
                         r=[PS[pi], B_const], w=[B_junkD, B_cnt])
            P.op("dve", lambda e: e.tensor_reduce(out=idxf[:, 0:2], in_=cnt[:, 0:8].rearrange("p (s t) -> p s t", s=2), axis=AX.X, op=ALU.add), r=[B_cnt], w=[B_cnt])
            P.op("dve", lambda e: e.tensor_scalar(out=idxf[:, 0:2], in0=idxf[:, 0:2], scalar1=float(S - 1), scalar2=None, op0=ALU.min), r=[B_cnt], w=[B_cnt])
            P.op("dve", lambda e, s_=s_: e.tensor_copy(out=idxi_t[:, 2 * s_:2 * s_ + 2], in_=idxf[:, 0:2]), r=[B_cnt], w=[B_idx[s_]])
            xe3 = xe[s_].rearrange("p (s n) -> p s n", s=2)
            for sc in range(2):
                P.op("pool", lambda e, s_=s_, sc=sc, xe3=xe3: e.indirect_dma_start(out=xe3[:, sc, :], out_offset=None, in_=hs_d,
                                                                               in_offset=bass.IndirectOffsetOnAxis(ap=idxi_t[:, 2 * s_ + sc:2 * s_ + sc + 1], axis=0)),
                     r=[B_idx[s_], B_hs], w=[B_xe[s_]], dma=True, sb=B_xe[s_])

        def emit_expert(ex):
            s_ = ex % 2
            xe3 = xe[s_].rearrange("p (s n) -> p s n", s=2)
            xeT3 = xeT[s_].rearrange("p (c n) -> p c n", c=8)
            for b4 in range(4):
                pi = next_ps(0, 2)
                for dd in range(2):
                    dc = b4 * 2 + dd
                    for sc in range(2):
                        P.op("pe", lambda e, pi=pi, dd=dd, sc=sc, dc=dc, xe3=xe3: e.transpose(out=psb[pi][:, (dd * 2 + sc) * 128:(dd * 2 + sc + 1) * 128], in_=xe3[:, sc, ts(dc, 128)], identity=ident),
                             r=[B_xe[s_], B_const], w=[PS[pi]])
                evac_copy(xeT[s_][:, b4 * 512:(b4 + 1) * 512], psb[pi][:, :], [PS[pi]], [B_xeT[s_]])
            for fb in range(4):
                wgv, bwg = wload(wg_d[ex].rearrange("(c p) n -> p c n", p=128)[:, :, ts(fb, 512)], 8)
                wuv, bwu = wload(wu_d[ex].rearrange("(c p) n -> p c n", p=128)[:, :, ts(fb, 512)], 8)
                for fi in range(4):
                    fc = fb * 4 + fi
                    pi = next_ps(2, 4)
                    for dc in range(8):
                        P.op("pe", lambda e, pi=pi, wgv=wgv, fi=fi, dc=dc, xeT3=xeT3: e.matmul(psb[pi][:, 0:256], lhsT=wgv[:, dc, ts(fi, 128)].bitcast(F32R), rhs=xeT3[:, dc, :].bitcast(F32R),
                                                                                        start=(dc == 0), stop=(dc == 7)),
                             r=[bwg, B_xeT[s_]], w=[PS[pi]])
                    for dc in range(8):
                        P.op("pe", lambda e, pi=pi, wuv=wuv, fi=fi, dc=dc, xeT3=xeT3: e.matmul(psb[pi][:, 256:512], lhsT=wuv[:, dc, ts(fi, 128)].bitcast(F32R), rhs=xeT3[:, dc, :].bitcast(F32R),
                                                                                        start=(dc == 0), stop=(dc == 7)),
                             r=[bwu, B_xeT[s_]], w=[PS[pi]])
                    gi = fc % 2
                    P.op("act", lambda e, pi=pi, gi=gi: e.activation(out=sg[gi], in_=psb[pi][:, 0:256], func=AF.Silu), r=[PS[pi]], w=[B_sg[gi]])
                    P.op("dve", lambda e, pi=pi, gi=gi, fc=fc: e.tensor_tensor(out=act3[:, fc, :].bitcast(F32R), in0=sg[gi], in1=psb[pi][:, 256:512], op=ALU.mult),
                         r=[PS[pi], B_sg[gi]], w=[B_act[fc]])
            for fb in range(4):
                wdv, bwd = wload(wd_d[ex].rearrange("(c p) n -> p c n", p=128)[:, fb * 4:(fb + 1) * 4, :], 4)
                for fi in range(4):
                    fc = fb * 4 + fi
                    for sc in range(2):
                        for half in range(2):
                            pi = 4 + sc * 2 + half
                            P.op("pe", lambda e, pi=pi, fc=fc, sc=sc, half=half, fi=fi, wdv=wdv: e.matmul(psb[pi][:, :], lhsT=act3[:, fc, ts(sc, 128)].bitcast(F32R), rhs=wdv[:, fi, ts(half, 512)].bitcast(F32R),
                                                                                                  start=(fc == 0), stop=(fc == 15)),
                                 r=[B_act[fc], bwd], w=[PS[pi]])
            ye3 = ye[s_].rearrange("p (s n) -> p s n", s=2)
            for sc in range(2):
                for half in range(2):
                    pi = 4 + sc * 2 + half
                    evac_scale(ye3[:, sc, ts(half, 512)], psb[pi][:, :], xe3[:, sc, D + ex:D + ex + 1], [PS[pi], B_xe[s_]], [B_ye[s_]])
            for sc in range(2):
                P.op("pool", lambda e, s_=s_, sc=sc, ye3=ye3: e.indirect_dma_start(out=acc_d, out_offset=bass.IndirectOffsetOnAxis(ap=idxi_t[:, 2 * s_ + sc:2 * s_ + sc + 1], axis=0),
                                                                               in_=ye3[:, sc, :], in_offset=None, compute_op=ALU.add, bounds_check=S - 1, oob_is_err=True),
                     r=[B_ye[s_], B_idx[s_]], w=[B_acc], dma=True, sb=B_ye[s_], waw=True)

        emit_idx(0)
        for ex in range(NE):
            if ex + 1 < NE:
                emit_idx(ex + 1)
            emit_expert(ex)

        if stop == 6:
            finish()
            return nc
        all_s6 = [B_acc, B_hs, B_cum, B_cume, B_cnt, B_affT, B_work] + B_aff + B_wr_ + B_xe + B_xeT + B_act + B_sg + B_ye + B_idx + PS + [B_wo, B_wr] + B_mT + B_xc + B_x1 + B_xs2 + B_h2T + B_st4 + [B_junkA, B_junkD]
        region_barrier(all_s6)
        reset(base_mark)
        wpg = allocR(8 * 1024)
        wpg3 = wpg.rearrange("p (c n) -> p c n", c=8)
        wpp = allocR(2 * 1024)
        wpp3 = wpp.rearrange("p (c n) -> p c n", c=2)
        gpost = allocF(1024)
        gple_rep = allocF(1024)
        B_gpr = Buf("gple_rep")
        P.op("sync", lambda e: e.dma_start(out=gple_rep, in_=grep_d[1]), w=[B_gpr], dma=True, sb=B_gpr)
        B_wpg = Buf("wpg")
        P.op("sync", lambda e: e.dma_start(out=wpg3.bitcast(F32R), in_=wpg_d.rearrange("(c p) n -> p c n", p=128)), w=[B_wpg], dma=True, sb=B_wpg)
        B_wpp = Buf("wpp")
        P.op("sync", lambda e: e.dma_start(out=wpp3.bitcast(F32R), in_=wpp_d.rearrange("(c p) n -> p c n", p=128)), w=[B_wpp], dma=True, sb=B_wpp)
        B_gpost = Buf("gpost")
        P.op("sync", lambda e: e.dma_start(out=gpost, in_=gpost_d), w=[B_gpost], dma=True, sb=B_gpost)
        x2c = [allocF(1024) for _ in range(3)]
        B_x2 = [Buf("x2c%d" % i) for i in range(3)]
        xs3 = [allocF(1024) for _ in range(2)]
        B_xs3 = [Buf("xs3_%d" % i) for i in range(2)]
        h3T = [allocR(8 * 128) for _ in range(2)]
        B_h3T = [Buf("h3T%d" % i) for i in range(2)]
        pTc = [allocR(2 * 128) for _ in range(2)]
        B_pT = [Buf("pTc%d" % i) for i in range(2)]
        sgm = [allocF(1024) for _ in range(2)]
        B_sgm = [Buf("sgm%d" % i) for i in range(2)]
        t1 = [allocF(1024) for _ in range(2)]
        B_t1 = [Buf("t1_%d" % i) for i in range(2)]
        st7 = allocF(64)
        B_st7 = [Buf("st7_%d" % i) for i in range(16)]
        P.op("pool", lambda e: e.memset(st7, 0.0), w=B_st7)
        pT_d3 = pT_d.rearrange("(c p) t -> p c t", p=128)
        def s7_front(c):
            s2 = c % 2
            pT3 = pTc[s2].rearrange("p (c n) -> p c n", c=2)
            P.op("sync", lambda e, c=c, s2=s2: e.dma_start(out=x2c[c % 3], in_=acc_d[ts(c, 128), :]), r=[B_acc], w=[B_x2[c % 3]], dma=True, sb=B_x2[c % 3])
            P.op("sync", lambda e, c=c, pT3=pT3: e.dma_start(out=pT3.bitcast(F32R), in_=pT_d3[:, :, ts(c, 128)]), w=[B_pT[s2]], dma=True, sb=B_pT[s2])
            P.op("act", lambda e, c=c, s2=s2: e.activation(out=junkA, in_=x2c[c % 3], func=AF.Square, accum_out=st7[:, c:c + 1]), r=[B_x2[c % 3]], w=[B_junkA, B_st7[c]])
            rstd_from_ss(st7[:, c:c + 1], st7[:, 16 + c:17 + c], [B_st7[c]], [B_st7[c]], D)
            P.op("dve", lambda e, c=c, s2=s2: e.scalar_tensor_tensor(out=xs3[s2], in0=x2c[c % 3], scalar=st7[:, 16 + c:17 + c], in1=gple_rep, op0=ALU.mult, op1=ALU.mult),
                 r=[B_x2[c % 3], B_st7[c], B_gpr], w=[B_xs3[s2]])
            pA, pB = 0, 1
            for dc in range(8):
                pi = pA if dc < 4 else pB
                P.op("pe", lambda e, pi=pi, dc=dc, s2=s2: e.transpose(out=psb[pi][:, ts(dc % 4, 128)], in_=xs3[s2][:, ts(dc, 128)], identity=ident),
                     r=[B_xs3[s2], B_const], w=[PS[pi]])
            evac_copy(h3T[s2][:, 0:512], psb[pA][:, :], [PS[pA]], [B_h3T[s2]])
            evac_copy(h3T[s2][:, 512:1024], psb[pB][:, :], [PS[pB]], [B_h3T[s2]])

        def s7_back(c):
            s2 = c % 2
            h3T3 = h3T[s2].rearrange("p (c n) -> p c n", c=8)
            pT3 = pTc[s2].rearrange("p (c n) -> p c n", c=2)
            pG = [2 + (c % 2) * 2, 3 + (c % 2) * 2]
            pE = [6, 7]
            for half in range(2):
                for kc in range(2):
                    P.op("pe", lambda e, half=half, kc=kc, pT3=pT3, pE=pE: e.matmul(psb[pE[half]][:, :], lhsT=pT3[:, kc, :].bitcast(F32R), rhs=wpp3[:, kc, ts(half, 512)].bitcast(F32R),
                                                                              start=(kc == 0), stop=(kc == 1)),
                         r=[B_pT[s2], B_wpp], w=[PS[pE[half]]])
            for half in range(2):
                P.op("act", lambda e, half=half, c=c, pE=pE: e.activation(out=junkA[:, 0:512], in_=psb[pE[half]][:, :], func=AF.Square, accum_out=st7[:, 32 + 16 * half + c:33 + 16 * half + c]),
                     r=[PS[pE[half]]], w=[B_junkA, B_st7[c]])
            for half in range(2):
                for dc in range(8):
                    P.op("pe", lambda e, half=half, dc=dc, h3T3=h3T3, pG=pG: e.matmul(psb[pG[half]][:, :], lhsT=h3T3[:, dc, :].bitcast(F32R), rhs=wpg3[:, dc, ts(half, 512)].bitcast(F32R),
                                                                                start=(dc == 0), stop=(dc == 7)),
                         r=[B_h3T[s2], B_wpg], w=[PS[pG[half]]])
            P.op("dve", lambda e, c=c: e.tensor_tensor(out=st7[:, 32 + c:33 + c], in0=st7[:, 32 + c:33 + c], in1=st7[:, 48 + c:49 + c], op=ALU.add), r=[B_st7[c]], w=[B_st7[c]])
            rstd_from_ss(st7[:, 32 + c:33 + c], st7[:, 48 + c:49 + c], [B_st7[c]], [B_st7[c]], D)
            for half in range(2):
                P.op("dve", lambda e, half=half, s2=s2, c=c, pE=pE: e.scalar_tensor_tensor(out=t1[s2][:, ts(half, 512)], in0=psb[pE[half]][:, :], scalar=st7[:, 48 + c:49 + c],
                                                                                     in1=gpost[:, ts(half, 512)], op0=ALU.mult, op1=ALU.mult),
                     r=[PS[pE[half]], B_st7[c], B_gpost], w=[B_t1[s2]])
            for half in range(2):
                P.op("act", lambda e, half=half, s2=s2, pG=pG: e.activation(out=sgm[s2][:, ts(half, 512)], in_=psb[pG[half]][:, :], func=AF.Sigmoid), r=[PS[pG[half]]], w=[B_sgm[s2]])
            P.op("dve", lambda e, s2=s2: e.tensor_tensor(out=t1[s2], in0=t1[s2], in1=sgm[s2], op=ALU.mult), r=[B_sgm[s2], B_t1[s2]], w=[B_t1[s2]])
            P.op("dve", lambda e, s2=s2: e.tensor_tensor(out=t1[s2], in0=t1[s2], in1=x2c[c % 3], op=ALU.add), r=[B_x2[c % 3], B_t1[s2]], w=[B_t1[s2]])
            P.op("pool", lambda e, c=c, s2=s2: e.dma_start(out=out_d[ts(c, 128), :], in_=t1[s2]), r=[B_t1[s2]], w=[B_out], dma=True, sb=B_t1[s2])

        s7_front(0)
        for c in range(NT):
            if c + 1 < NT:
                s7_front(c + 1)
            s7_back(c)
        finish()
    return nc


_NC_CACHE = {}


def _bias_tiles(rpb):
    H = rpb.shape[0]
    tiles = np.full((H, 20, 128, 512), NEG, dtype=np.float32)
    a = np.arange(128) // 64
    kc = np.arange(128) % 64
    b = np.arange(512) // 64
    qc = np.arange(512) % 64
    cs = np.clip(qc - 8, 0, 48)
    for qb, chunks in ((0, QB_CHUNKS[0]), (1, QB_CHUNKS[1]), (3, QB_CHUNKS[3])):
        r = 8 * qb + b
        rs = np.clip(r - 4, 0, 24)
        for j in chunks:
            kr = 2 * j + a
            vr = (kr[:, None] >= rs[None, :]) & (kr[:, None] <= rs[None, :] + 7)
            vc = (kc[:, None] >= cs[None, :]) & (kc[:, None] <= cs[None, :] + 15)
            valid = vr & vc
            dr = np.clip(kr[:, None] - r[None, :] + 7, 0, 14)
            dc = np.clip(kc[:, None] - qc[None, :] + 15, 0, 30)
            g = rpb[:, dr, dc]
            tiles[:, bias_tile_id(qb, j)] = np.where(valid[None], g, np.float32(NEG))
    return tiles


def kernel(x, p, norm_mix, w_in, w_pool, pool_scale, q_norm, k_norm, rpb, w_out, norm_ffn, w_router,
           w_gate, w_up, w_down, norm_ple, w_ple_gate, w_ple_proj, norm_ple_post):
    f = lambda a: np.ascontiguousarray(np.asarray(a, dtype=np.float32))
    x = f(x); p = f(p)
    B = x.shape[0]
    if "nc" not in _NC_CACHE:
        _NC_CACHE["nc"] = build_nc()
    nc = _NC_CACHE["nc"]
    col = lambda v: f(v).reshape(-1, 128).T
    gcols = np.zeros((128, 32), np.float32)
    gcols[:, 0:8] = col(norm_mix[0])
    gcols[:, 8:16] = col(norm_ffn[0])
    gcols[:, 16:24] = col(norm_ple[0])
    gcols[:, 24:28] = col(pool_scale[0])
    gcols[:, 28] = np.tile(f(q_norm[0]), 2)
    gcols[:, 29] = np.tile(f(k_norm[0]), 2)
    gpost = np.ascontiguousarray(np.broadcast_to(f(norm_ple_post[0])[None, :], (128, D)))
    bias_tiles = _bias_tiles(f(rpb[0]))
    grep = np.ascontiguousarray(np.stack([np.broadcast_to(f(norm_ffn[0])[None, :], (128, D)), np.broadcast_to(f(norm_ple[0])[None, :], (128, D))], axis=0))
    shared = dict(w_in=f(w_in[0]), w_pool=f(w_pool[0]), w_out=f(w_out[0]), w_router=f(w_router[0]),
                  w_gate=f(w_gate[0]), w_up=f(w_up[0]), w_down=f(w_down[0]), w_ple_gate=f(w_ple_gate[0]),
                  w_ple_proj=f(w_ple_proj[0]), gcols=gcols, gpost=gpost, bias_tiles=bias_tiles, grep=grep)
    in_maps = []
    for b in range(B):
        m = dict(shared)
        m["x"] = x[b]
        m["pT"] = np.ascontiguousarray(p[0, b].T)
        in_maps.append(m)
    res = run_bass_kernel_spmd(nc, in_maps, core_ids=list(range(B)))
    return np.stack([np.asarray(r["out"], dtype=np.float32) for r in res.results], axis=0)
```

```python
import numpy as np
from contextlib import ExitStack
import concourse.bass as bass
import concourse.mybir as mybir
from concourse.bass_utils import run_bass_kernel_spmd

F32 = mybir.dt.float32
F32R = mybir.dt.float32r
I32 = mybir.dt.int32
ALU = mybir.AluOpType
AF = mybir.ActivationFunctionType
AX = mybir.AxisListType

S = 2048
D = 1024
NT = 16
NE = 16
CAP = 256
DFF = 2048
EPS = 1e-6
NEG = -30000.0
HSW = D + NE

QB_CHUNKS = {0: list(range(0, 6)), 1: list(range(2, 10)), 2: list(range(6, 14)), 3: list(range(10, 16))}


def bias_tile_id(qb, j):
    if qb == 0:
        return j
    if qb == 1:
        return 6 + (j - 2)
    if qb == 2:
        return 6 + (j - 6)
    return 14 + (j - 10)


def ts(i, n):
    return slice(i * n, (i + 1) * n)


class Buf:
    def __init__(self, name):
        self.name = name
        self.writes = []
        self.reads = []
        self.sem = None
        self.cnt = 0


class Op:
    __slots__ = ("eng", "fn", "deps", "dma", "sem", "val", "need_sig", "sigval")


class Prog:
    ENG = ("sync", "act", "pe", "dve", "pool")

    def __init__(self, nc, ctx):
        self.nc = nc
        self.ctx = ctx
        self.q = {k: [] for k in self.ENG}
        self.prog_sem = {k: ctx.enter_context(nc.semaphore("prog_" + k)) for k in self.ENG}
        self.nsem = 5

    def op(self, eng, fn, r=(), w=(), dma=False, sb=None, waw=False, part=False):
        o = Op()
        o.eng = eng
        o.fn = fn
        o.dma = dma
        o.need_sig = False
        o.sigval = None
        o.sem = None
        o.val = None
        raw = []
        war = []
        for b in r:
            raw.extend(b.writes)
        for b in w:
            if b.reads:
                war.extend(b.reads)
                war.extend(b.writes)
            elif not part:
                war.extend(b.writes)
        for b in w:
            if b.reads:
                b.reads = []
                b.writes = []
            b.writes.append(o)
        for b in r:
            if b not in w:
                b.reads.append(o)
        kept = []
        seen = set()
        for d in raw:
            if id(d) in seen or d is o:
                continue
            seen.add(id(d))
            if (not d.dma) and d.eng == eng and eng == "pe" and not dma:
                continue
            if not d.dma:
                d.need_sig = True
            kept.append(d)
        for d in war:
            if id(d) in seen or d is o:
                continue
            seen.add(id(d))
            if (not d.dma) and d.eng == eng and not dma:
                continue
            if not d.dma:
                d.need_sig = True
            kept.append(d)
        o.deps = kept
        if dma:
            assert sb is not None
            if sb.sem is None:
                sb.sem = self.ctx.enter_context(self.nc.semaphore("d_" + sb.name))
                self.nsem += 1
            sb.cnt += 16
            o.sem = sb.sem
            o.val = sb.cnt
        self.q[eng].append(o)
        return o

    def barrier(self, bufs):
        deps = []
        seen = set()
        for b in bufs:
            for d in list(b.writes) + list(b.reads):
                if id(d) not in seen and d.fn is not None:
                    seen.add(id(d))
                    deps.append(d)
        for k in self.ENG:
            o = Op()
            o.eng = k; o.fn = None; o.dma = False; o.need_sig = False; o.sigval = None; o.sem = None; o.val = None
            kept = []
            for d in deps:
                if (not d.dma) and d.eng == k:
                    continue
                if not d.dma:
                    d.need_sig = True
                kept.append(d)
            o.deps = kept
            self.q[k].append(o)

    def run(self):
        for k in self.ENG:
            c = 0
            for o in self.q[k]:
                if (not o.dma) and o.need_sig:
                    c += 1
                    o.sigval = c
        nc = self.nc
        engs = {"sync": None, "act": None, "pe": None, "dve": None, "pool": None}

        def emit(k, e):
            waited = {}
            for o in self.q[k]:
                need = {}
                for d in o.deps:
                    if d.dma:
                        key, v = d.sem, d.val
                    else:
                        key, v = self.prog_sem[d.eng], d.sigval
                    kk = id(key)
                    if kk not in need or need[kk][1] < v:
                        need[kk] = (key, v)
                for kk, (key, v) in need.items():
                    if waited.get(kk, 0) >= v:
                        continue
                    waited[kk] = v
                    e.wait_ge(key, v)
                if o.fn is None:
                    continue
                ins = o.fn(e)
                if o.dma:
                    ins.then_inc(o.sem, 16)
                elif o.need_sig:
                    ins.then_inc(self.prog_sem[k], 1)

        with nc.Block() as block:
            @block.sync
            def _(e):
                emit("sync", e)

            @block.scalar
            def _(e):
                emit("act", e)

            @block.tensor
            def _(e):
                emit("pe", e)

            @block.vector
            def _(e):
                emit("dve", e)

            @block.gpsimd
            def _(e):
                emit("pool", e)


class _Stop(Exception):
    pass


def build_nc(stop=99, debug=False):
    nc = bass.Bass("TRN2", target_bir_lowering=False)
    nc.dge_precook = False
    dt_in = lambda name, shape, dt=F32R: nc.dram_tensor(name, shape, dt, kind="ExternalInput").ap()
    x_d = dt_in("x", [S, D], F32)
    pT_d = dt_in("pT", [256, S])
    win_d = dt_in("w_in", [D, 2048])
    wpool_d = dt_in("w_pool", [4, 128, 128])
    wout_d = dt_in("w_out", [D, D])
    wr_d = dt_in("w_router", [D, NE])
    wg_d = dt_in("w_gate", [NE, D, DFF])
    wu_d = dt_in("w_up", [NE, D, DFF])
    wd_d = dt_in("w_down", [NE, DFF, D])
    wpg_d = dt_in("w_ple_gate", [D, D])
    wpp_d = dt_in("w_ple_proj", [256, D])
    gcols_d = dt_in("gcols", [128, 32], F32)
    gpost_d = dt_in("gpost", [128, D], F32)
    grep_d = dt_in("grep", [2, 128, D], F32)
    bias_d = dt_in("bias_tiles", [8, 20, 128, 512])
    out_d = nc.dram_tensor("out", [S, D], F32, kind="ExternalOutput").ap()
    skind = "ExternalOutput" if debug else "Internal"
    zT_d = nc.dram_tensor("zT_s", [1536, S], F32R, kind=skind).ap()
    v_d = nc.dram_tensor("v_s", [S, 512], F32R, kind=skind).ap()
    mix_d = nc.dram_tensor("mix_s", [D, S], F32R, kind=skind).ap()
    hs_d = nc.dram_tensor("hs_s", [S, HSW], F32, kind=skind).ap()
    acc_d = nc.dram_tensor("acc_s", [S, D], F32, kind=skind).ap()

    with ExitStack() as ctx:
        P = Prog(nc, ctx)
        COLS_R = 31500
        COLS_F = 19700
        bigR = ctx.enter_context(nc.sbuf_tensor("bigR", [128, COLS_R], F32R))
        bigF = ctx.enter_context(nc.sbuf_tensor("bigF", [128, COLS_F], F32))
        cur = {"R": 0, "F": 0}

        def allocR(n):
            a = cur["R"]
            cur["R"] += n
            assert cur["R"] <= COLS_R, ("R", cur["R"])
            return bigR[:, a:a + n]

        def allocF(n):
            a = cur["F"]
            cur["F"] += n
            assert cur["F"] <= COLS_F, ("F", cur["F"])
            return bigF[:, a:a + n]

        def mark():
            return dict(cur)

        def reset(m):
            cur.update(m)

        idxi_t = ctx.enter_context(nc.sbuf_tensor("idxi", [128, 4], I32))
        psb = [ctx.enter_context(nc.psum_tensor("ps%d" % i, [128, 512], F32)) for i in range(8)]
        PS = [Buf("ps%d" % i) for i in range(8)]

        ident = allocF(128)
        gcols = allocF(32)
        slotid = allocF(2)
        junkA = allocF(1024)
        junkD = allocF(512)
        B_const = Buf("const")
        B_gcols = Buf("gcols")
        B_junkA = Buf("junkA")
        B_junkD = Buf("junkD")
        P.op("sync", lambda e: e.dma_start(out=gcols, in_=gcols_d), w=[B_gcols], dma=True, sb=B_gcols)
        B_cb = Buf("cbuild")
        cb = allocF(640)
        aff_all = allocF(256)
        P.op("pool", lambda e: e.memset(ident, 1.0), w=[B_cb, B_const])
        P.op("pool", lambda e: e.affine_select(out=ident, in_=ident, pattern=[[-1, 128]], compare_op=ALU.is_equal,
                                               fill=0.0, base=0, channel_multiplier=1), w=[B_cb, B_const])
        P.op("pool", lambda e: e.iota(slotid, pattern=[[128, 2]], base=0, channel_multiplier=1,
                                      allow_small_or_imprecise_dtypes=True), w=[B_cb, B_const])
        P.op("pool", lambda e: e.memset(cb, 0.0), w=[B_cb])
        P.op("pool", lambda e: e.tensor_copy(out=cb[:, 0:128], in_=ident), w=[B_cb])
        P.op("pool", lambda e: e.memset(cb[0:64, 128:192], 1.0 / 64), w=[B_cb])
        P.op("pool", lambda e: e.memset(cb[64:128, 192:256], 1.0 / 64), w=[B_cb])
        P.op("pool", lambda e: e.memset(cb[:, 256:320], 1.0), w=[B_cb])
        P.op("pool", lambda e: e.memset(cb[:, 448:512], 1.0), w=[B_cb])
        P.op("pool", lambda e: e.memset(cb[:, 512:640], 1.0), w=[B_cb])
        crr = allocR(640)
        onesall = crr[:, 512:640]
        identr = crr[:, 0:128]
        blk64 = crr[:, 128:256]
        onesA = crr[:, 256:384]
        onesB = crr[:, 384:512]
        P.op("dve", lambda e: e.tensor_copy(out=crr.bitcast(F32R), in_=cb), r=[B_cb], w=[B_const])
        B_zT = Buf("zT"); B_v = Buf("v_d"); B_mix = Buf("mix_d"); B_acc = Buf("acc_d"); B_hs = Buf("hs_d"); B_out = Buf("out")
        DRAMB = [B_zT, B_v, B_mix, B_acc, B_hs, B_out]

        def finish():
            P.op("pool", None, r=DRAMB)
            P.op("sync", None, r=DRAMB)
            P.run()

        base_mark = mark()

        def rstd_from_ss(ss_col, out_col, bufs_r, bufs_w, n):
            P.op("act", lambda e: e.activation(out=out_col, in_=ss_col, func=AF.Ln, bias=EPS, scale=1.0 / n), r=bufs_r, w=bufs_w)
            P.op("act", lambda e: e.activation(out=out_col, in_=out_col, func=AF.Exp, scale=-0.5), r=bufs_w, w=bufs_w)

        evac_flip = [0]

        def evac_scale(out_ap, in_ap, scal_ap, r, w):
            evac_flip[0] ^= 1
            if evac_flip[0]:
                return P.op("act", lambda e: e.activation(out=out_ap, in_=in_ap, func=AF.Copy, scale=scal_ap), r=r, w=w, part=True)
            return P.op("dve", lambda e: e.tensor_scalar(out=out_ap, in0=in_ap, scalar1=scal_ap, scalar2=None, op0=ALU.mult), r=r, w=w, part=True)

        def evac_copy(out_ap, in_ap, r, w):
            evac_flip[0] ^= 1
            if evac_flip[0]:
                return P.op("act", lambda e: e.activation(out=out_ap, in_=in_ap, func=AF.Copy), r=r, w=w, part=True)
            return P.op("dve", lambda e: e.tensor_copy(out=out_ap, in_=in_ap), r=r, w=w, part=True)

        win = allocR(8 * 2048)
        win3 = win.rearrange("p (c n) -> p c n", c=8)
        B_win = [Buf("win%d" % i) for i in range(8)]
        xc = [allocF(1024) for _ in range(8)]
        B_xc = [Buf("xc%d" % i) for i in range(8)]
        stat = allocF(64)
        B_stat = [Buf("stat%d" % i) for i in range(16)]
        P.op("pool", lambda e: e.memset(stat, 0.0), w=B_stat)
        hTb = [allocR(8 * 512) for _ in range(2)]
        B_hT = [[Buf("hT%d_%d" % (s_, dc)) for dc in range(8)] for s_ in range(2)]
        stg = [allocR(512) for _ in range(4)]
        B_stg = [Buf("stg%d" % i) for i in range(4)]
        stg_i = [0]
        ps_i = [0]

        def next_ps(lo, hi):
            i = lo + (ps_i[0] % (hi - lo))
            ps_i[0] += 1
            return i

        def store(out_dram, stg_ap, stg_buf, bdram, eng="pool"):
            P.op(eng, lambda e: e.dma_start(out=out_dram, in_=stg_ap.bitcast(F32R)), r=[stg_buf], w=[bdram],
                 dma=True, sb=stg_buf)

        def s1_front_chunk(tb, j):
            xo = (tb % 2) * 4
            c = tb * 4 + j
            xj = xc[xo + j]
            P.op("sync", lambda e, c=c, xj=xj: e.dma_start(out=xj, in_=x_d[ts(c, 128), :]), w=[B_xc[xo + j]], dma=True, sb=B_xc[xo + j])
            P.op("act", lambda e, c=c, xj=xj: e.activation(out=junkA, in_=xj, func=AF.Square, accum_out=stat[:, c:c + 1]),
                 r=[B_xc[xo + j]], w=[B_junkA, B_stat[c]])
            rstd_from_ss(stat[:, c:c + 1], stat[:, 16 + c:17 + c], [B_stat[c]], [B_stat[c]], D)
            P.op("act", lambda e, c=c, xj=xj: e.activation(out=xj, in_=xj, func=AF.Copy, scale=stat[:, 16 + c:17 + c]),
                 r=[B_xc[xo + j], B_stat[c]], w=[B_xc[xo + j]])

        def s1_front(tb):
            for j in range(4):
                s1_front_chunk(tb, j)

        def s1_back(tb):
            hs_ = tb % 2
            hT3 = hTb[hs_].rearrange("p (c n) -> p c n", c=8)
            xo = (tb % 2) * 4
            for dc in range(8):
                pi = next_ps(0, 2)
                for j in range(4):
                    P.op("pe", lambda e, pi=pi, j=j, dc=dc, xj=xc[xo + j]: e.transpose(out=psb[pi][:, ts(j, 128)], in_=xj[:, ts(dc, 128)], identity=ident),
                         r=[B_xc[xo + j], B_const], w=[PS[pi]])
                evac_scale(hT3[:, dc, :].bitcast(F32R), psb[pi][:, :], gcols[:, dc:dc + 1], [PS[pi], B_gcols], [B_hT[hs_][dc]])
            for n in range(12):
                pi = next_ps(2, 8)
                for dc in range(8):
                    P.op("pe", lambda e, pi=pi, n=n, dc=dc, hT3=hT3: e.matmul(psb[pi][:, :], lhsT=win3[:, dc, ts(n, 128)].bitcast(F32R),
                                                                          rhs=hT3[:, dc, :].bitcast(F32R), start=(dc == 0), stop=(dc == 7)),
                         r=[B_win[dc], B_hT[hs_][dc]], w=[PS[pi]])
                si = stg_i[0] % 4
                stg_i[0] += 1
                evac_copy(stg[si].bitcast(F32R), psb[pi][:, :], [PS[pi]], [B_stg[si]])
                store(zT_d[ts(n, 128), ts(tb, 512)], stg[si], B_stg[si], B_zT)
                if tb + 1 < 4 and n % 3 == 2:
                    s1_front_chunk(tb + 1, n // 3)
            for j in range(4):
                c = tb * 4 + j
                pi = next_ps(2, 8)
                for dc in range(8):
                    P.op("pe", lambda e, pi=pi, j=j, dc=dc, hT3=hT3: e.matmul(psb[pi][:, :], lhsT=hT3[:, dc, ts(j, 128)].bitcast(F32R),
                                                                          rhs=win3[:, dc, 1536:2048].bitcast(F32R), start=(dc == 0), stop=(dc == 7)),
                         r=[B_win[dc], B_hT[hs_][dc]], w=[PS[pi]])
                si = stg_i[0] % 4
                stg_i[0] += 1
                evac_copy(stg[si].bitcast(F32R), psb[pi][:, :], [PS[pi]], [B_stg[si]])
                store(v_d[ts(c, 128), :], stg[si], B_stg[si], B_v)

        s1_front(0)
        for dc in range(8):
            P.op("sync", lambda e, dc=dc: e.dma_start(out=win3[:, dc, :].bitcast(F32R), in_=win_d[ts(dc, 128), :]),
                 w=[B_win[dc]], dma=True, sb=B_win[dc])
        for tb in range(4):
            s1_back(tb)

        if stop == 1:
            finish()
            return nc
        reset(base_mark)
        stg = [allocR(512) for _ in range(4)]
        B_region = Buf("region")

        def region_barrier(bufs):
            P.barrier(bufs)

        all_s1 = B_win + [B_zT, B_v] + B_xc + B_stat + B_hT[0] + B_hT[1] + B_stg + PS + [B_junkA]
        region_barrier(all_s1)
        B_stg = [Buf("stg2_%d" % i) for i in range(4)]
        upad = [allocF(2064) for _ in range(2)]
        B_up = [Buf("upad%d" % i) for i in range(2)]
        ta = allocF(2064)
        tb_ = allocF(2064)
        rc = allocF(2048)
        dn = allocR(2048)
        wpool = allocR(4 * 128)
        wpool3 = wpool.rearrange("p (g n) -> p g n", g=4)
        B_ta = Buf("ta")
        B_tb = Buf("tb")
        B_rc = Buf("rc")
        B_dn = Buf("dn")
        B_wpool = Buf("wpool")
        P.op("sync", lambda e: e.dma_start(out=wpool3.bitcast(F32R), in_=wpool_d.rearrange("g c n -> c g n")), w=[B_wpool], dma=True, sb=B_wpool)
        for i in range(2):
            P.op("pool", lambda e, i=i: e.memset(upad[i], 0.0), w=[B_up[i]])
        def emit_pool_dve(g):
            wsz = (2, 4, 8, 16)[g]
            u = upad[g % 2]
            bu = B_up[g % 2]
            P.op("sync", lambda e, g=g, u=u: e.dma_start(out=u[:, 8:2056].bitcast(F32R), in_=zT_d[ts(g, 128), :]), r=[B_zT], w=[bu], dma=True, sb=bu)
            hw = wsz // 2
            P.op("pool", lambda e, wsz=wsz: e.memset(rc, 1.0 / wsz), w=[B_rc])
            for t in range(hw):
                P.op("pool", lambda e, t=t, hw=hw: e.memset(rc[:, t:t + 1], 1.0 / (t + hw)), w=[B_rc])
            for t in range(S - hw + 1, S):
                P.op("pool", lambda e, t=t, hw=hw: e.memset(rc[:, t:t + 1], 1.0 / (S - t + hw)), w=[B_rc])
            P.op("pool", lambda e, u=u: e.tensor_tensor(out=ta[:, 0:2063], in0=u[:, 0:2063], in1=u[:, 1:2064], op=ALU.add), r=[bu], w=[B_ta])
            src, bsrc, width = ta, B_ta, 2063
            other, bother = tb_, B_tb
            step = 2
            while step < wsz:
                nw = width - step
                P.op("pool", lambda e, src=src, other=other, nw=nw, step=step: e.tensor_tensor(out=other[:, 0:nw], in0=src[:, 0:nw], in1=src[:, step:step + nw], op=ALU.add),
                     r=[bsrc], w=[bother])
                src, bsrc, other, bother = other, bother, src, bsrc
                width = nw
                step *= 2
            off = 8 - hw
            P.op("pool", lambda e, src=src, other=other, off=off: e.tensor_tensor(out=other[:, 0:2048], in0=src[:, off:off + 2048], in1=rc, op=ALU.mult),
                 r=[bsrc, B_rc], w=[bother])
            P.op("pool", lambda e, other=other, u=u: e.tensor_tensor(out=dn.bitcast(F32R), in0=other[:, 0:2048], in1=u[:, 8:2056], op=ALU.subtract),
                 r=[bother, bu], w=[B_dn])

        def emit_pool_mm(g):
            for tb in range(4):
                pi = 7
                P.op("pe", lambda e, pi=pi, g=g, tb=tb: e.matmul(psb[pi][:, :], lhsT=wpool3[:, g, :].bitcast(F32R), rhs=dn[:, ts(tb, 512)].bitcast(F32R), start=True, stop=True),
                     r=[B_wpool, B_dn], w=[PS[pi]])
                si = stg_i[0] % 4
                stg_i[0] += 1
                evac_scale(stg[si].bitcast(F32R), psb[pi][:, :], gcols[:, 24 + g:25 + g], [PS[pi], B_gcols], [B_stg[si]])
                store(mix_d[ts(g, 128), ts(tb, 512)], stg[si], B_stg[si], B_mix)

        if stop == 2:
            for g in range(4):
                emit_pool_dve(g)
                emit_pool_mm(g)
            finish()
            return nc
        qraw = [allocF(2048)] * 2
        kraw = [allocF(2048)] * 2
        B_qraw = [Buf("qraw")] * 2
        B_kraw = [Buf("kraw")] * 2
        sq = allocR(2048)
        B_sq = Buf("sq")
        rsd = allocF(512)
        B_rsd = Buf("rsd")
        qn = [allocR(2048) for _ in range(2)]
        knA = [allocR(2048) for _ in range(2)]
        knB = [allocR(2048) for _ in range(2)]
        B_qn = [Buf("qn%d" % i) for i in range(2)]
        B_kn = [Buf("kn%d" % i) for i in range(2)]
        vA = allocR(16 * 128)
        vB = allocR(16 * 128)
        vA3 = vA.rearrange("p (c n) -> p c n", c=16)
        vB3 = vB.rearrange("p (c n) -> p c n", c=16)
        B_vA = Buf("vA")
        B_vB = Buf("vB")
        NBT = 8
        bt = [allocR(512) for _ in range(NBT)]
        B_bt = [Buf("bt%d" % i) for i in range(NBT)]
        Pt = [allocR(512) for _ in range(3)]
        B_Pt = [Buf("Pt%d" % i) for i in range(3)]
        rD = allocF(512)
        B_rD = Buf("rD")
        tmpS = [allocF(512) for _ in range(3)]
        B_tmpS = [Buf("tmpS%d" % i) for i in range(3)]
        for q4 in range(4):
            for i2 in range(2):
                P.op("dve", lambda e, q4=q4, i2=i2: e.tensor_scalar(out=knA[i2][64:128, ts(q4, 512)], in0=cb[64:128, 0:512], scalar1=0.0, scalar2=None, op0=ALU.mult), r=[B_cb], w=[B_kn[i2]])
                P.op("dve", lambda e, q4=q4, i2=i2: e.tensor_scalar(out=knB[i2][0:64, ts(q4, 512)], in0=cb[0:64, 0:512], scalar1=0.0, scalar2=None, op0=ALU.mult), r=[B_cb], w=[B_kn[i2]])
        for q4 in range(4):
            P.op("dve", lambda e, q4=q4: e.tensor_scalar(out=vA[:, ts(q4, 512)], in0=cb[:, 0:512], scalar1=0.0, scalar2=None, op0=ALU.mult), r=[B_cb], w=[B_vA])
            P.op("dve", lambda e, q4=q4: e.tensor_scalar(out=vB[:, ts(q4, 512)], in0=cb[:, 0:512], scalar1=0.0, scalar2=None, op0=ALU.mult), r=[B_cb], w=[B_vB])
        v_d3 = v_d.rearrange("(c p) n -> p c n", p=128)
        bt_i = [0]
        pt_i = [0]
        for pr in range(4):
            sl = pr % 2
            P.op("sync", lambda e, pr=pr, sl=sl: e.dma_start(out=qraw[sl].bitcast(F32R), in_=zT_d[512 + pr * 128:512 + (pr + 1) * 128, :]), r=[B_zT], w=[B_qraw[sl]], dma=True, sb=B_qraw[sl])
            P.op("sync", lambda e, pr=pr, sl=sl: e.dma_start(out=kraw[sl].bitcast(F32R), in_=zT_d[1024 + pr * 128:1024 + (pr + 1) * 128, :]), r=[B_zT], w=[B_kraw[sl]], dma=True, sb=B_kraw[sl])
            P.op("sync", lambda e, pr=pr: e.dma_start(out=vA3[:, :, 0:64].bitcast(F32R), in_=v_d3[:, :, pr * 128:pr * 128 + 64]), r=[B_v], w=[B_vA], dma=True, sb=B_vA)
            P.op("sync", lambda e, pr=pr: e.dma_start(out=vB3[:, :, 64:128].bitcast(F32R), in_=v_d3[:, :, pr * 128 + 64:pr * 128 + 128]), r=[B_v], w=[B_vB], dma=True, sb=B_vB)
            for which in range(2):
                raw = (qraw, kraw)[which][sl]
                braw = (B_qraw, B_kraw)[which][sl]
                dst = qn[sl]
                bdst = (B_qn, B_kn)[which][sl]
                gcol = gcols[:, 28 + which:29 + which]
                P.op("act", lambda e, raw=raw: e.activation(out=sq.bitcast(F32R), in_=raw, func=AF.Square), r=[braw], w=[B_sq])
                for tb in range(4):
                    pi = next_ps(7, 8)
                    P.op("pe", lambda e, pi=pi, tb=tb: e.matmul(psb[pi][:, :], lhsT=blk64.bitcast(F32R), rhs=sq[:, ts(tb, 512)].bitcast(F32R), start=True, stop=True),
                         r=[B_const, B_sq], w=[PS[pi]])
                    sc_ = 64.0 if which == 0 else 1.0
                    P.op("act", lambda e, pi=pi, sc_=sc_: e.activation(out=rsd, in_=psb[pi][:, :], func=AF.Ln, bias=EPS * sc_, scale=sc_), r=[PS[pi]], w=[B_rsd])
                    P.op("act", lambda e: e.activation(out=rsd, in_=rsd, func=AF.Exp, scale=-0.5), r=[B_rsd], w=[B_rsd])
                    if which == 0:
                        P.op("dve", lambda e, raw=raw, dst=dst, tb=tb, gcol=gcol: e.scalar_tensor_tensor(out=dst[:, ts(tb, 512)].bitcast(F32R), in0=raw[:, ts(tb, 512)], scalar=gcol,
                                                                                                     in1=rsd, op0=ALU.mult, op1=ALU.mult),
                             r=[braw, B_rsd, B_gcols], w=[bdst])
                    else:
                        for (lo, hi, kdst) in ((0, 64, knA[sl]), (64, 128, knB[sl])):
                            P.op("dve", lambda e, raw=raw, kdst=kdst, tb=tb, lo=lo, hi=hi: e.scalar_tensor_tensor(out=kdst[lo:hi, ts(tb, 512)].bitcast(F32R), in0=raw[lo:hi, ts(tb, 512)],
                                                                                                          scalar=gcols[lo:hi, 29:30], in1=rsd[lo:hi, :], op0=ALU.mult, op1=ALU.mult),
                                 r=[braw, B_rsd, B_gcols], w=[bdst])
            emit_pool_dve(pr)
            steps = [(qb, hh, j) for qb in range(4) for hh in range(2) for j in QB_CHUNKS[qb]]
            nst = len(steps)
            bslot = {}
            qk_info = {}

            def emit_bdma(si_):
                qb, hh, j = steps[si_]
                h = pr * 2 + hh
                bi = bt_i[0] % NBT
                bt_i[0] += 1
                bslot[si_] = bi
                P.op("sync", lambda e, h=h, qb=qb, j=j, bi=bi: e.dma_start(out=bt[bi].bitcast(F32R), in_=bias_d[h, bias_tile_id(qb, j), :, :]), w=[B_bt[bi]], dma=True, sb=B_bt[bi])

            def emit_qk(si_):
                qb, hh, j = steps[si_]
                pi = next_ps(0, 3)
                bi = bslot[si_]
                hb = 64 * hh
                kpad = knA[sl] if hh == 0 else knB[sl]
                P.op("pe", lambda e, pi=pi, kpad=kpad, j=j, qb=qb, sl=sl: e.matmul(psb[pi][:, :], lhsT=kpad[:, ts(j, 128)].bitcast(F32R),
                                                                             rhs=qn[sl][:, ts(qb, 512)].bitcast(F32R), start=True, stop=True),
                     r=[B_kn[sl], B_qn[sl]], w=[PS[pi]])
                ti = pt_i[0] % 3
                pt_i[0] += 1
                P.op("dve", lambda e, pi=pi, bi=bi, ti=ti: e.tensor_tensor(out=tmpS[ti], in0=psb[pi][:, :], in1=bt[bi].bitcast(F32), op=ALU.add),
                     r=[PS[pi], B_bt[bi]], w=[B_tmpS[ti]])
                P.op("act", lambda e, ti=ti: e.activation(out=Pt[ti].bitcast(F32R), in_=tmpS[ti], func=AF.Exp), r=[B_tmpS[ti]], w=[B_Pt[ti]])
                qk_info[si_] = ti

            def emit_pv(si_):
                qb, hh, j = steps[si_]
                nps, dps = (3, 4) if (qb % 2 == 0) else (5, 6)
                ti = qk_info[si_]
                first = (si_ == 0) or (steps[si_ - 1][0] != qb)
                last = (si_ == nst - 1) or (steps[si_ + 1][0] != qb)
                vt3 = vA3 if hh == 0 else vB3
                bv = B_vA if hh == 0 else B_vB
                on = onesA if hh == 0 else onesB
                extra = []
                P.op("pe", lambda e, vt3=vt3, j=j, ti=ti, first=first, last=last, nps=nps: e.matmul(psb[nps][:, :], lhsT=vt3[:, j, :].bitcast(F32R), rhs=Pt[ti].bitcast(F32R), start=first, stop=last),
                     r=[bv, B_Pt[ti]] + extra, w=[PS[nps]])
                P.op("pe", lambda e, on=on, ti=ti, first=first, last=last, dps=dps: e.matmul(psb[dps][:, :], lhsT=on.bitcast(F32R), rhs=Pt[ti].bitcast(F32R), start=first, stop=last),
                     r=[B_const, B_Pt[ti]], w=[PS[dps]])
                if last:
                    P.op("act", lambda e, dps=dps: e.activation(out=rD, in_=psb[dps][:, :], func=AF.Ln), r=[PS[dps]], w=[B_rD])
                    P.op("act", lambda e: e.activation(out=rD, in_=rD, func=AF.Exp, scale=-1.0), r=[B_rD], w=[B_rD])
                    si = stg_i[0] % 4
                    stg_i[0] += 1
                    P.op("dve", lambda e, nps=nps, si=si: e.tensor_tensor(out=stg[si].bitcast(F32R), in0=psb[nps][:, :], in1=rD, op=ALU.mult), r=[PS[nps], B_rD], w=[B_stg[si]])
                    store(mix_d[512 + pr * 128:512 + (pr + 1) * 128, ts(qb, 512)], stg[si], B_stg[si], B_mix)

            for si_ in range(min(NBT - 1, nst)):
                emit_bdma(si_)
            emit_qk(0)
            if nst > 1:
                emit_qk(1)
            for si_ in range(nst):
                if si_ + NBT - 1 < nst:
                    emit_bdma(si_ + NBT - 1)
                if si_ + 2 < nst:
                    emit_qk(si_ + 2)
                emit_pv(si_)
            emit_pool_mm(pr)

        if stop == 3:
            finish()
            return nc
        all_s3 = (B_tmpS + [B_zT, B_v, B_mix, B_sq, B_rsd, B_vA, B_vB, B_rD, B_ta, B_tb, B_rc, B_dn, B_wpool] + B_up + B_qraw + B_kraw + B_qn + B_kn + B_bt + B_Pt + B_stg + PS)
        region_barrier(all_s3)
        reset(base_mark)
        wo = allocR(8 * 1024)
        wo3 = wo.rearrange("p (c n) -> p c n", c=8)
        B_wo = Buf("wo")
        P.op("sync", lambda e: e.dma_start(out=wo3.bitcast(F32R), in_=wout_d.rearrange("(c p) n -> p c n", p=128)), w=[B_wo], dma=True, sb=B_wo)
        wr = allocR(8 * 16)
        wr3 = wr.rearrange("p (c n) -> p c n", c=8)
        B_wr = Buf("wr")
        P.op("sync", lambda e: e.dma_start(out=wr3.bitcast(F32R), in_=wr_d.rearrange("(c p) n -> p c n", p=128)), w=[B_wr], dma=True, sb=B_wr)
        gffn_rep = allocF(1024)
        B_gfr = Buf("gffn_rep")
        P.op("sync", lambda e: e.dma_start(out=gffn_rep, in_=grep_d[0]), w=[B_gfr], dma=True, sb=B_gfr)
        mT = [allocR(8 * 128) for _ in range(2)]
        B_mT = [Buf("mT%d" % i) for i in range(2)]
        xc = [allocF(1024) for _ in range(2)]
        B_xc = [Buf("xc4_%d" % i) for i in range(2)]
        x1c = [allocF(1024) for _ in range(2)]
        B_x1 = [Buf("x1c%d" % i) for i in range(2)]
        xs2 = [allocF(HSW) for _ in range(3)]
        B_xs2 = [Buf("xs2_%d" % i) for i in range(3)]
        h2T = [allocR(8 * 128) for _ in range(2)]
        B_h2T = [Buf("h2T%d" % i) for i in range(2)]
        aff3 = aff_all.rearrange("p (c n) -> p c n", c=16)
        B_aff = [Buf("aff%d" % i) for i in range(16)]
        st4 = allocF(64)
        B_st4 = [Buf("st4_%d" % i) for i in range(16)]
        P.op("pool", lambda e: e.memset(st4, 0.0), w=B_st4)
        mix_d3 = mix_d.rearrange("(c p) t -> p c t", p=128)
        def s4_front(c):
            s2 = c % 2
            mT3 = mT[s2].rearrange("p (c n) -> p c n", c=8)
            P.op("sync", lambda e, c=c, mT3=mT3: e.dma_start(out=mT3.bitcast(F32R), in_=mix_d3[:, :, ts(c, 128)]), r=[B_mix], w=[B_mT[s2]], dma=True, sb=B_mT[s2])
            P.op("sync", lambda e, c=c, s2=s2: e.dma_start(out=xc[s2], in_=x_d[ts(c, 128), :]), w=[B_xc[s2]], dma=True, sb=B_xc[s2])
            for half in range(2):
                pi = half
                for k in range(8):
                    P.op("pe", lambda e, pi=pi, k=k, half=half, mT3=mT3: e.matmul(psb[pi][:, :], lhsT=mT3[:, k, :].bitcast(F32R), rhs=wo3[:, k, ts(half, 512)].bitcast(F32R),
                                                                            start=(k == 0), stop=(k == 7)),
                         r=[B_mT[s2], B_wo], w=[PS[pi]])
                P.op("dve", lambda e, pi=pi, half=half, s2=s2: e.tensor_tensor(out=x1c[s2][:, ts(half, 512)], in0=psb[pi][:, :], in1=xc[s2][:, ts(half, 512)], op=ALU.add),
                     r=[PS[pi], B_xc[s2]], w=[B_x1[s2]])
            P.op("pool", lambda e, c=c, s2=s2: e.dma_start(out=acc_d[ts(c, 128), :], in_=x1c[s2]), r=[B_x1[s2]], w=[B_acc], dma=True, sb=B_x1[s2])
            P.op("act", lambda e, c=c, s2=s2: e.activation(out=junkA, in_=x1c[s2], func=AF.Square, accum_out=st4[:, c:c + 1]), r=[B_x1[s2]], w=[B_junkA, B_st4[c]])
            rstd_from_ss(st4[:, c:c + 1], st4[:, 16 + c:17 + c], [B_st4[c]], [B_st4[c]], D)
            P.op("dve", lambda e, c=c, s2=s2: e.scalar_tensor_tensor(out=xs2[c % 3][:, 0:D], in0=x1c[s2], scalar=st4[:, 16 + c:17 + c], in1=gffn_rep, op0=ALU.mult, op1=ALU.mult),
                 r=[B_x1[s2], B_st4[c], B_gfr], w=[B_xs2[c % 3]])
            pA = 4 + 2 * s2
            pB = 5 + 2 * s2
            for dc in range(8):
                pi = pA if dc < 4 else pB
                P.op("pe", lambda e, pi=pi, dc=dc, s2=s2: e.transpose(out=psb[pi][:, ts(dc % 4, 128)], in_=xs2[c % 3][:, ts(dc, 128)], identity=ident),
                     r=[B_xs2[c % 3], B_const], w=[PS[pi]])
            evac_copy(h2T[s2][:, 0:512], psb[pA][:, :], [PS[pA]], [B_h2T[s2]])
            evac_copy(h2T[s2][:, 512:1024], psb[pB][:, :], [PS[pB]], [B_h2T[s2]])

        def s4_back(c):
            s2 = c % 2
            h2T3 = h2T[s2].rearrange("p (c n) -> p c n", c=8)
            pr_ = 2 + s2
            for dc in range(8):
                P.op("pe", lambda e, pr_=pr_, dc=dc, h2T3=h2T3: e.matmul(psb[pr_][:, 0:16], lhsT=h2T3[:, dc, :].bitcast(F32R), rhs=wr3[:, dc, :].bitcast(F32R), start=(dc == 0), stop=(dc == 7)),
                     r=[B_h2T[s2], B_wr], w=[PS[pr_]])
            P.op("dve", lambda e, pr_=pr_, c=c: e.tensor_reduce(out=st4[:, 32 + c:33 + c], in_=psb[pr_][:, 0:16], axis=AX.X, op=ALU.max), r=[PS[pr_]], w=[B_st4[c]])
            P.op("dve", lambda e, c=c: e.tensor_scalar(out=st4[:, 32 + c:33 + c], in0=st4[:, 32 + c:33 + c], scalar1=-1.0, scalar2=None, op0=ALU.mult), r=[B_st4[c]], w=[B_st4[c]])
            P.op("act", lambda e, pr_=pr_, c=c: e.activation(out=aff3[:, c, :], in_=psb[pr_][:, 0:16], func=AF.Exp, bias=st4[:, 32 + c:33 + c], scale=1.0, accum_out=st4[:, 48 + c:49 + c]),
                 r=[PS[pr_], B_st4[c]], w=[B_aff[c], B_st4[c]])
            P.op("dve", lambda e, c=c: e.reciprocal(out=st4[:, 48 + c:49 + c], in_=st4[:, 48 + c:49 + c]), r=[B_st4[c]], w=[B_st4[c]])
            P.op("dve", lambda e, c=c: e.tensor_scalar(out=aff3[:, c, :], in0=aff3[:, c, :], scalar1=st4[:, 48 + c:49 + c], scalar2=None, op0=ALU.mult), r=[B_st4[c], B_aff[c]], w=[B_aff[c]])
            P.op("dve", lambda e, c=c, s2=s2: e.tensor_copy(out=xs2[c % 3][:, D:HSW], in_=aff3[:, c, :]), r=[B_aff[c]], w=[B_xs2[c % 3]])
            P.op("pool", lambda e, c=c, s2=s2: e.dma_start(out=hs_d[ts(c, 128), :], in_=xs2[c % 3]), r=[B_xs2[c % 3]], w=[B_hs], dma=True, sb=B_xs2[c % 3])

        s4_front(0)
        for c in range(NT):
            if c + 1 < NT:
                s4_front(c + 1)
            s4_back(c)

        if stop == 4:
            finish()
            return nc
        all_s4 = [B_wo, B_wr, B_mix, B_junkA] + B_mT + B_xc + B_x1 + B_xs2 + B_h2T + B_st4 + PS
        region_barrier(all_s4)
        reset(base_mark)
        xe_all = allocF(2 * 2 * HSW)
        affT = xe_all[:, 0:2048]
        work = xe_all[:, 2048:4096]
        cum = allocR(2048)
        cume = allocR(2048)
        B_cume = Buf("cume")
        mx8 = allocF(8)
        B_affT = Buf("affT")
        B_work = Buf("work")
        B_cum = Buf("cum")
        for tb in range(4):
            pi = next_ps(0, 4)
            for j in range(4):
                c = tb * 4 + j
                P.op("pe", lambda e, pi=pi, j=j, c=c: e.matmul(psb[pi][0:16, ts(j, 128)], lhsT=aff3[:, c, :], rhs=ident, start=True, stop=True),
                     r=[B_aff[c], B_const], w=[PS[pi]])
            P.op("dve", lambda e, pi=pi, tb=tb: e.tensor_copy(out=affT[0:16, ts(tb, 512)], in_=psb[pi][0:16, :]), r=[PS[pi]], w=[B_affT])
        for rnd in range(CAP // 8):
            srcw = affT if rnd == 0 else work
            P.op("dve", lambda e, srcw=srcw: e.max(out=mx8[0:16, :], in_=srcw[0:16, :]), r=[B_affT, B_work], w=[B_work])
            if rnd < CAP // 8 - 1:
                P.op("dve", lambda e, srcw=srcw: e.match_replace(out=work[0:16, :], in_to_replace=mx8[0:16, :], in_values=srcw[0:16, :], imm_value=-1.0),
                     r=[B_affT, B_work], w=[B_work])
        P.op("dve", lambda e: e.tensor_scalar(out=work[0:16, :], in0=affT[0:16, :], scalar1=mx8[0:16, 7:8], scalar2=None, op0=ALU.is_ge), r=[B_affT, B_work], w=[B_work])
        P.op("dve", lambda e: e.tensor_tensor_scan(out=cum[0:16, :].bitcast(F32R), data0=work[0:16, :], data1=work[0:16, :], initial=0.0, op0=ALU.add, op1=ALU.max),
             r=[B_work], w=[B_cum])

        moe_mark = mark()
        NW = 5
        wring = [allocR(4096) for _ in range(NW)]
        B_wr_ = [Buf("wring%d" % i) for i in range(NW)]
        xe = [xe_all[:, 0:2 * HSW], xe_all[:, 2 * HSW:4 * HSW]]
        B_xe = [Buf("xe%d" % i) for i in range(2)]
        xeT = [allocR(8 * 256)] * 2
        B_xeT = [Buf("xeT")] * 2
        actb = allocR(16 * 256)
        act3 = actb.rearrange("p (c n) -> p c n", c=16)
        B_act = [Buf("act%d" % i) for i in range(16)]
        sg = [allocF(256) for _ in range(2)]
        B_sg = [Buf("sg%d" % i) for i in range(2)]
        ye = [allocF(2 * 1024) for _ in range(2)]
        B_ye = [Buf("ye%d" % i) for i in range(2)]
        cnt = allocF(16)
        idxf = allocF(4)
        B_idx = [Buf("idx%d" % i) for i in range(2)]
        B_cnt = Buf("cnt")
        wi = [0]

        def wload(dram_ap, shape_c):
            i = wi[0] % NW
            wi[0] += 1
            view = wring[i].rearrange("p (c n) -> p c n", c=shape_c)
            P.op("sync", lambda e, view=view, dram_ap=dram_ap: e.dma_start(out=view.bitcast(F32R), in_=dram_ap), w=[B_wr_[i]], dma=True, sb=B_wr_[i])
            return view, B_wr_[i]

        def emit_idx(ex):
            s_ = ex % 2
            P.op("dve", lambda e, ex=ex: e.tensor_scalar(out=cume[0:16, :], in0=cum[0:16, :].bitcast(F32), scalar1=ident[0:16, ex:ex + 1], scalar2=None, op0=ALU.mult),
                 r=[B_cum, B_const], w=[B_cume])
            for tb in range(4):
                pi = next_ps(0, 2)
                P.op("pe", lambda e, pi=pi, tb=tb: e.matmul(psb[pi][:, :], lhsT=onesall[0:16, :], rhs=cume[0:16, ts(tb, 512)], start=True, stop=True),
                     r=[B_const, B_cume], w=[PS[pi]])
                for sc in range(2):
                    P.op("dve", lambda e, pi=pi, tb=tb, sc=sc: e.tensor_scalar(out=junkD, in0=psb[pi][:, :], scalar1=slotid[:, sc:sc + 1], scalar2=0.0, op0=ALU.is_le, op1=ALU.add,
                                                                         accum_out=cnt[:, sc * 4 + tb:sc * 4 + tb + 1]),
                         r=[PS[pi], B_const], w=[B_junkD, B_cnt])
            P.op("dve", lambda e: e.tensor_reduce(out=idxf[:, 0:2], in_=cnt[:, 0:8].rearrange("p (s t) -> p s t", s=2), axis=AX.X, op=ALU.add), r=[B_cnt], w=[B_cnt])
            P.op("dve", lambda e: e.tensor_scalar(out=idxf[:, 0:2], in0=idxf[:, 0:2], scalar1=float(S - 1), scalar2=None, op0=ALU.min), r=[B_cnt], w=[B_cnt])
            P.op("dve", lambda e, s_=s_: e.tensor_copy(out=idxi_t[:, 2 * s_:2 * s_ + 2], in_=idxf[:, 0:2]), r=[B_cnt], w=[B_idx[s_]])
            xe3 = xe[s_].rearrange("p (s n) -> p s n", s=2)
            for sc in range(2):
                P.op("pool", lambda e, s_=s_, sc=sc, xe3=xe3: e.indirect_dma_start(out=xe3[:, sc, :], out_offset=None, in_=hs_d,
                                                                               in_offset=bass.IndirectOffsetOnAxis(ap=idxi_t[:, 2 * s_ + sc:2 * s_ + sc + 1], axis=0)),
                     r=[B_idx[s_], B_hs], w=[B_xe[s_]], dma=True, sb=B_xe[s_])

        def emit_expert(ex):
            s_ = ex % 2
            xe3 = xe[s_].rearrange("p (s n) -> p s n", s=2)
            xeT3 = xeT[s_].rearrange("p (c n) -> p c n", c=8)
            for b4 in range(4):
                pi = next_ps(0, 2)
                for dd in range(2):
                    dc = b4 * 2 + dd
                    for sc in range(2):
                        P.op("pe", lambda e, pi=pi, dd=dd, sc=sc, dc=dc, xe3=xe3: e.transpose(out=psb[pi][:, (dd * 2 + sc) * 128:(dd * 2 + sc + 1) * 128], in_=xe3[:, sc, ts(dc, 128)], identity=ident),
                             r=[B_xe[s_], B_const], w=[PS[pi]])
                evac_copy(xeT[s_][:, b4 * 512:(b4 + 1) * 512], psb[pi][:, :], [PS[pi]], [B_xeT[s_]])
            for fb in range(4):
                wgv, bwg = wload(wg_d[ex].rearrange("(c p) n -> p c n", p=128)[:, :, ts(fb, 512)], 8)
                wuv, bwu = wload(wu_d[ex].rearrange("(c p) n -> p c n", p=128)[:, :, ts(fb, 512)], 8)
                for fi in range(4):
                    fc = fb * 4 + fi
                    pi = next_ps(2, 4)
                    for dc in range(8):
                        P.op("pe", lambda e, pi=pi, wgv=wgv, fi=fi, dc=dc, xeT3=xeT3: e.matmul(psb[pi][:, 0:256], lhsT=wgv[:, dc, ts(fi, 128)].bitcast(F32R), rhs=xeT3[:, dc, :].bitcast(F32R),
                                                                                        start=(dc == 0), stop=(dc == 7)),
                             r=[bwg, B_xeT[s_]], w=[PS[pi]])
                    for dc in range(8):
                        P.op("pe", lambda e, pi=pi, wuv=wuv, fi=fi, dc=dc, xeT3=xeT3: e.matmul(psb[pi][:, 256:512], lhsT=wuv[:, dc, ts(fi, 128)].bitcast(F32R), rhs=xeT3[:, dc, :].bitcast(F32R),
                                                                                        start=(dc == 0), stop=(dc == 7)),
                             r=[bwu, B_xeT[s_]], w=[PS[pi]])
                    gi = fc % 2
                    P.op("act", lambda e, pi=pi, gi=gi: e.activation(out=sg[gi], in_=psb[pi][:, 0:256], func=AF.Silu), r=[PS[pi]], w=[B_sg[gi]])
                    P.op("dve", lambda e, pi=pi, gi=gi, fc=fc: e.tensor_tensor(out=act3[:, fc, :].bitcast(F32R), in0=sg[gi], in1=psb[pi][:, 256:512], op=ALU.mult),
                         r=[PS[pi], B_sg[gi]], w=[B_act[fc]])
            for fb in range(4):
                wdv, bwd = wload(wd_d[ex].rearrange("(c p) n -> p c n", p=128)[:, fb * 4:(fb + 1) * 4, :], 4)
                for fi in range(4):
                    fc = fb * 4 + fi
                    for sc in range(2):
                        for half in range(2):
                            pi = 4 + sc * 2 + half
                            P.op("pe", lambda e, pi=pi, fc=fc, sc=sc, half=half, fi=fi, wdv=wdv: e.matmul(psb[pi][:, :], lhsT=act3[:, fc, ts(sc, 128)].bitcast(F32R), rhs=wdv[:, fi, ts(half, 512)].bitcast(F32R),
                                                                                                  start=(fc == 0), stop=(fc == 15)),
                                 r=[B_act[fc], bwd], w=[PS[pi]])
            ye3 = ye[s_].rearrange("p (s n) -> p s n", s=2)
            for sc in range(2):
                for half in range(2):
                    pi = 4 + sc * 2 + half
                    evac_scale(ye3[:, sc, ts(half, 512)], psb[pi][:, :], xe3[:, sc, D + ex:D + ex + 1], [PS[pi], B_xe[s_]], [B_ye[s_]])
            for sc in range(2):
                P.op("pool", lambda e, s_=s_, sc=sc, ye3=ye3: e.indirect_dma_start(out=acc_d, out_offset=bass.IndirectOffsetOnAxis(ap=idxi_t[:, 2 * s_ + sc:2 * s_ + sc + 1], axis=0),
                                                                               in_=ye3[:, sc, :], in_offset=None, compute_op=ALU.add, bounds_check=S - 1, oob_is_err=True),
                     r=[B_ye[s_], B_idx[s_]], w=[B_acc], dma=True, sb=B_ye[s_], waw=True)

        emit_idx(0)
        for ex in range(NE):
            if ex + 1 < NE:
                emit_idx(ex + 1)
            emit_expert(ex)

        if stop == 6:
            finish()
            return nc
        all_s6 = [B_acc, B_hs, B_cum, B_cume, B_cnt, B_affT, B_work] + B_aff + B_wr_ + B_xe + B_xeT + B_act + B_sg + B_ye + B_idx + PS + [B_wo, B_wr] + B_mT + B_xc + B_x1 + B_xs2 + B_h2T + B_st4 + [B_junkA, B_junkD]
        region_barrier(all_s6)
        reset(base_mark)
        wpg = allocR(8 * 1024)
        wpg3 = wpg.rearrange("p (c n) -> p c n", c=8)
        wpp = allocR(2 * 1024)
        wpp3 = wpp.rearrange("p (c n) -> p c n", c=2)
        gpost = allocF(1024)
        gple_rep = allocF(1024)
        B_gpr = Buf("gple_rep")
        P.op("sync", lambda e: e.dma_start(out=gple_rep, in_=grep_d[1]), w=[B_gpr], dma=True, sb=B_gpr)
        B_wpg = Buf("wpg")
        P.op("sync", lambda e: e.dma_start(out=wpg3.bitcast(F32R), in_=wpg_d.rearrange("(c p) n -> p c n", p=128)), w=[B_wpg], dma=True, sb=B_wpg)
        B_wpp = Buf("wpp")
        P.op("sync", lambda e: e.dma_start(out=wpp3.bitcast(F32R), in_=wpp_d.rearrange("(c p) n -> p c n", p=128)), w=[B_wpp], dma=True, sb=B_wpp)
        B_gpost = Buf("gpost")
        P.op("sync", lambda e: e.dma_start(out=gpost, in_=gpost_d), w=[B_gpost], dma=True, sb=B_gpost)
        x2c = [allocF(1024) for _ in range(3)]
        B_x2 = [Buf("x2c%d" % i) for i in range(3)]
        xs3 = [allocF(1024) for _ in range(2)]
        B_xs3 = [Buf("xs3_%d" % i) for i in range(2)]
        h3T = [allocR(8 * 128) for _ in range(2)]
        B_h3T = [Buf("h3T%d" % i) for i in range(2)]
        pTc = [allocR(2 * 128) for _ in range(2)]
        B_pT = [Buf("pTc%d" % i) for i in range(2)]
        sgm = [allocF(1024) for _ in range(2)]
        B_sgm = [Buf("sgm%d" % i) for i in range(2)]
        t1 = [allocF(1024) for _ in range(2)]
        B_t1 = [Buf("t1_%d" % i) for i in range(2)]
        st7 = allocF(64)
        B_st7 = [Buf("st7_%d" % i) for i in range(16)]
        P.op("pool", lambda e: e.memset(st7, 0.0), w=B_st7)
        pT_d3 = pT_d.rearrange("(c p) t -> p c t", p=128)
        def s7_front(c):
            s2 = c % 2
            pT3 = pTc[s2].rearrange("p (c n) -> p c n", c=2)
            P.op("sync", lambda e, c=c, s2=s2: e.dma_start(out=x2c[c % 3], in_=acc_d[ts(c, 128), :]), r=[B_acc], w=[B_x2[c % 3]], dma=True, sb=B_x2[c % 3])
            P.op("sync", lambda e, c=c, pT3=pT3: e.dma_start(out=pT3.bitcast(F32R), in_=pT_d3[:, :, ts(c, 128)]), w=[B_pT[s2]], dma=True, sb=B_pT[s2])
            P.op("act", lambda e, c=c, s2=s2: e.activation(out=junkA, in_=x2c[c % 3], func=AF.Square, accum_out=st7[:, c:c + 1]), r=[B_x2[c % 3]], w=[B_junkA, B_st7[c]])
            rstd_from_ss(st7[:, c:c + 1], st7[:, 16 + c:17 + c], [B_st7[c]], [B_st7[c]], D)
            P.op("dve", lambda e, c=c, s2=s2: e.scalar_tensor_tensor(out=xs3[s2], in0=x2c[c % 3], scalar=st7[:, 16 + c:17 + c], in1=gple_rep, op0=ALU.mult, op1=ALU.mult),
                 r=[B_x2[c % 3], B_st7[c], B_gpr], w=[B_xs3[s2]])
            pA, pB = 0, 1
            for dc in range(8):
                pi = pA if dc < 4 else pB
                P.op("pe", lambda e, pi=pi, dc=dc, s2=s2: e.transpose(out=psb[pi][:, ts(dc % 4, 128)], in_=xs3[s2][:, ts(dc, 128)], identity=ident),
                     r=[B_xs3[s2], B_const], w=[PS[pi]])
            evac_copy(h3T[s2][:, 0:512], psb[pA][:, :], [PS[pA]], [B_h3T[s2]])
            evac_copy(h3T[s2][:, 512:1024], psb[pB][:, :], [PS[pB]], [B_h3T[s2]])

        def s7_back(c):
            s2 = c % 2
            h3T3 = h3T[s2].rearrange("p (c n) -> p c n", c=8)
            pT3 = pTc[s2].rearrange("p (c n) -> p c n", c=2)
            pG = [2 + (c % 2) * 2, 3 + (c % 2) * 2]
            pE = [6, 7]
            for half in range(2):
                for kc in range(2):
                    P.op("pe", lambda e, half=half, kc=kc, pT3=pT3, pE=pE: e.matmul(psb[pE[half]][:, :], lhsT=pT3[:, kc, :].bitcast(F32R), rhs=wpp3[:, kc, ts(half, 512)].bitcast(F32R),
                                                                              start=(kc == 0), stop=(kc == 1)),
                         r=[B_pT[s2], B_wpp], w=[PS[pE[half]]])
            for half in range(2):
                P.op("act", lambda e, half=half, c=c, pE=pE: e.activation(out=junkA[:, 0:512], in_=psb[pE[half]][:, :], func=AF.Square, accum_out=st7[:, 32 + 16 * half + c:33 + 16 * half + c]),
                     r=[PS[pE[half]]], w=[B_junkA, B_st7[c]])
            for half in range(2):
                for dc in range(8):
                    P.op("pe", lambda e, half=half, dc=dc, h3T3=h3T3, pG=pG: e.matmul(psb[pG[half]][:, :], lhsT=h3T3[:, dc, :].bitcast(F32R), rhs=wpg3[:, dc, ts(half, 512)].bitcast(F32R),
                                                                                start=(dc == 0), stop=(dc == 7)),
                         r=[B_h3T[s2], B_wpg], w=[PS[pG[half]]])
            P.op("dve", lambda e, c=c: e.tensor_tensor(out=st7[:, 32 + c:33 + c], in0=st7[:, 32 + c:33 + c], in1=st7[:, 48 + c:49 + c], op=ALU.add), r=[B_st7[c]], w=[B_st7[c]])
            rstd_from_ss(st7[:, 32 + c:33 + c], st7[:, 48 + c:49 + c], [B_st7[c]], [B_st7[c]], D)
            for half in range(2):
                P.op("dve", lambda e, half=half, s2=s2, c=c, pE=pE: e.scalar_tensor_tensor(out=t1[s2][:, ts(half, 512)], in0=psb[pE[half]][:, :], scalar=st7[:, 48 + c:49 + c],
                                                                                     in1=gpost[:, ts(half, 512)], op0=ALU.mult, op1=ALU.mult),
                     r=[PS[pE[half]], B_st7[c], B_gpost], w=[B_t1[s2]])
            for half in range(2):
                P.op("act", lambda e, half=half, s2=s2, pG=pG: e.activation(out=sgm[s2][:, ts(half, 512)], in_=psb[pG[half]][:, :], func=AF.Sigmoid), r=[PS[pG[half]]], w=[B_sgm[s2]])
            P.op("dve", lambda e, s2=s2: e.tensor_tensor(out=t1[s2], in0=t1[s2], in1=sgm[s2], op=ALU.mult), r=[B_sgm[s2], B_t1[s2]], w=[B_t1[s2]])
            P.op("dve", lambda e, s2=s2: e.tensor_tensor(out=t1[s2], in0=t1[s2], in1=x2c[c % 3], op=ALU.add), r=[B_x2[c % 3], B_t1[s2]], w=[B_t1[s2]])
            P.op("pool", lambda e, c=c, s2=s2: e.dma_start(out=out_d[ts(c, 128), :], in_=t1[s2]), r=[B_t1[s2]], w=[B_out], dma=True, sb=B_t1[s2])

        s7_front(0)
        for c in range(NT):
            if c + 1 < NT:
                s7_front(c + 1)
            s7_back(c)
        finish()
    return nc


_NC_CACHE = {}


def _bias_tiles(rpb):
    H = rpb.shape[0]
    tiles = np.full((H, 20, 128, 512), NEG, dtype=np.float32)
    a = np.arange(128) // 64
    kc = np.arange(128) % 64
    b = np.arange(512) // 64
    qc = np.arange(512) % 64
    cs = np.clip(qc - 8, 0, 48)
    for qb, chunks in ((0, QB_CHUNKS[0]), (1, QB_CHUNKS[1]), (3, QB_CHUNKS[3])):
        r = 8 * qb + b
        rs = np.clip(r - 4, 0, 24)
        for j in chunks:
            kr = 2 * j + a
            vr = (kr[:, None] >= rs[None, :]) & (kr[:, None] <= rs[None, :] + 7)
            vc = (kc[:, None] >= cs[None, :]) & (kc[:, None] <= cs[None, :] + 15)
            valid = vr & vc
            dr = np.clip(kr[:, None] - r[None, :] + 7, 0, 14)
            dc = np.clip(kc[:, None] - qc[None, :] + 15, 0, 30)
            g = rpb[:, dr, dc]
            tiles[:, bias_tile_id(qb, j)] = np.where(valid[None], g, np.float32(NEG))
    return tiles


def kernel(x, p, norm_mix, w_in, w_pool, pool_scale, q_norm, k_norm, rpb, w_out, norm_ffn, w_router,
           w_gate, w_up, w_down, norm_ple, w_ple_gate, w_ple_proj, norm_ple_post):
    f = lambda a: np.ascontiguousarray(np.asarray(a, dtype=np.float32))
    x = f(x); p = f(p)
    B = x.shape[0]
    if "nc" not in _NC_CACHE:
        _NC_CACHE["nc"] = build_nc()
    nc = _NC_CACHE["nc"]
    col = lambda v: f(v).reshape(-1, 128).T
    gcols = np.zeros((128, 32), np.float32)
    gcols[:, 0:8] = col(norm_mix[0])
    gcols[:, 8:16] = col(norm_ffn[0])
    gcols[:, 16:24] = col(norm_ple[0])
    gcols[:, 24:28] = col(pool_scale[0])
    gcols[:, 28] = np.tile(f(q_norm[0]), 2)
    gcols[:, 29] = np.tile(f(k_norm[0]), 2)
    gpost = np.ascontiguousarray(np.broadcast_to(f(norm_ple_post[0])[None, :], (128, D)))
    bias_tiles = _bias_tiles(f(rpb[0]))
    grep = np.ascontiguousarray(np.stack([np.broadcast_to(f(norm_ffn[0])[None, :], (128, D)), np.broadcast_to(f(norm_ple[0])[None, :], (128, D))], axis=0))
    shared = dict(w_in=f(w_in[0]), w_pool=f(w_pool[0]), w_out=f(w_out[0]), w_router=f(w_router[0]),
                  w_gate=f(w_gate[0]), w_up=f(w_up[0]), w_down=f(w_down[0]), w_ple_gate=f(w_ple_gate[0]),
                  w_ple_proj=f(w_ple_proj[0]), gcols=gcols, gpost=gpost, bias_tiles=bias_tiles, grep=grep)
    in_maps = []
    for b in range(B):
        m = dict(shared)
        m["x"] = x[b]
        m["pT"] = np.ascontiguousarray(p[0, b].T)
        in_maps.append(m)
    res = run_bass_kernel_spmd(nc, in_maps, core_ids=list(range(B)))
    return np.stack([np.asarray(r["out"], dtype=np.float32) for r in res.results], axis=0)
```

```python
import numpy as np
from contextlib import ExitStack
import concourse.bass as bass
import concourse.mybir as mybir
from concourse.bass_utils import run_bass_kernel_spmd

F32 = mybir.dt.float32
F32R = mybir.dt.float32r
I32 = mybir.dt.int32
ALU = mybir.AluOpType
AF = mybir.ActivationFunctionType
AX = mybir.AxisListType

S = 2048
D = 1024
NT = 16
NE = 16
CAP = 256
DFF = 2048
EPS = 1e-6
NEG = -30000.0
HSW = D + NE

QB_CHUNKS = {0: list(range(0, 6)), 1: list(range(2, 10)), 2: list(range(6, 14)), 3: list(range(10, 16))}


def bias_tile_id(qb, j):
    if qb == 0:
        return j
    if qb == 1:
        return 6 + (j - 2)
    if qb == 2:
        return 6 + (j - 6)
    return 14 + (j - 10)


def ts(i, n):
    return slice(i * n, (i + 1) * n)


class Buf:
    def __init__(self, name):
        self.name = name
        self.writes = []
        self.reads = []
        self.sem = None
        self.cnt = 0


class Op:
    __slots__ = ("eng", "fn", "deps", "dma", "sem", "val", "need_sig", "sigval")


class Prog:
    ENG = ("sync", "act", "pe", "dve", "pool")

    def __init__(self, nc, ctx):
        self.nc = nc
        self.ctx = ctx
        self.q = {k: [] for k in self.ENG}
        self.prog_sem = {k: ctx.enter_context(nc.semaphore("prog_" + k)) for k in self.ENG}
        self.nsem = 5

    def op(self, eng, fn, r=(), w=(), dma=False, sb=None, waw=False, part=False):
        o = Op()
        o.eng = eng
        o.fn = fn
        o.dma = dma
        o.need_sig = False
        o.sigval = None
        o.sem = None
        o.val = None
        raw = []
        war = []
        for b in r:
            raw.extend(b.writes)
        for b in w:
            if b.reads:
                war.extend(b.reads)
                war.extend(b.writes)
            elif not part:
                war.extend(b.writes)
        for b in w:
            if b.reads:
                b.reads = []
                b.writes = []
            b.writes.append(o)
        for b in r:
            if b not in w:
                b.reads.append(o)
        kept = []
        seen = set()
        for d in raw:
            if id(d) in seen or d is o:
                continue
            seen.add(id(d))
            if (not d.dma) and d.eng == eng and eng == "pe" and not dma:
                continue
            if not d.dma:
                d.need_sig = True
            kept.append(d)
        for d in war:
            if id(d) in seen or d is o:
                continue
            seen.add(id(d))
            if (not d.dma) and d.eng == eng and not dma:
                continue
            if not d.dma:
                d.need_sig = True
            kept.append(d)
        o.deps = kept
        if dma:
            assert sb is not None
            if sb.sem is None:
                sb.sem = self.ctx.enter_context(self.nc.semaphore("d_" + sb.name))
                self.nsem += 1
            sb.cnt += 16
            o.sem = sb.sem
            o.val = sb.cnt
        self.q[eng].append(o)
        return o

    def barrier(self, bufs):
        deps = []
        seen = set()
        for b in bufs:
            for d in list(b.writes) + list(b.reads):
                if id(d) not in seen and d.fn is not None:
                    seen.add(id(d))
                    deps.append(d)
        for k in self.ENG:
            o = Op()
            o.eng = k; o.fn = None; o.dma = False; o.need_sig = False; o.sigval = None; o.sem = None; o.val = None
            kept = []
            for d in deps:
                if (not d.dma) and d.eng == k:
                    continue
                if not d.dma:
                    d.need_sig = True
                kept.append(d)
            o.deps = kept
            self.q[k].append(o)

    def run(self):
        for k in self.ENG:
            c = 0
            for o in self.q[k]:
                if (not o.dma) and o.need_sig:
                    c += 1
                    o.sigval = c
        nc = self.nc
        engs = {"sync": None, "act": None, "pe": None, "dve": None, "pool": None}

        def emit(k, e):
            waited = {}
            for o in self.q[k]:
                need = {}
                for d in o.deps:
                    if d.dma:
                        key, v = d.sem, d.val
                    else:
                        key, v = self.prog_sem[d.eng], d.sigval
                    kk = id(key)
                    if kk not in need or need[kk][1] < v:
                        need[kk] = (key, v)
                for kk, (key, v) in need.items():
                    if waited.get(kk, 0) >= v:
                        continue
                    waited[kk] = v
                    e.wait_ge(key, v)
                if o.fn is None:
                    continue
                ins = o.fn(e)
                if o.dma:
                    ins.then_inc(o.sem, 16)
                elif o.need_sig:
                    ins.then_inc(self.prog_sem[k], 1)

        with nc.Block() as block:
            @block.sync
            def _(e):
                emit("sync", e)

            @block.scalar
            def _(e):
                emit("act", e)

            @block.tensor
            def _(e):
                emit("pe", e)

            @block.vector
            def _(e):
                emit("dve", e)

            @block.gpsimd
            def _(e):
                emit("pool", e)


class _Stop(Exception):
    pass


def build_nc(stop=99, debug=False):
    nc = bass.Bass("TRN2", target_bir_lowering=False)
    nc.dge_precook = False
    dt_in = lambda name, shape, dt=F32R: nc.dram_tensor(name, shape, dt, kind="ExternalInput").ap()
    x_d = dt_in("x", [S, D], F32)
    pT_d = dt_in("pT", [256, S])
    win_d = dt_in("w_in", [D, 2048])
    wpool_d = dt_in("w_pool", [4, 128, 128])
    wout_d = dt_in("w_out", [D, D])
    wr_d = dt_in("w_router", [D, NE])
    wg_d = dt_in("w_gate", [NE, D, DFF])
    wu_d = dt_in("w_up", [NE, D, DFF])
    wd_d = dt_in("w_down", [NE, DFF, D])
    wpg_d = dt_in("w_ple_gate", [D, D])
    wpp_d = dt_in("w_ple_proj", [256, D])
    gcols_d = dt_in("gcols", [128, 32], F32)
    gpost_d = dt_in("gpost", [128, D], F32)
    grep_d = dt_in("grep", [2, 128, D], F32)
    bias_d = dt_in("bias_tiles", [8, 20, 128, 512])
    out_d = nc.dram_tensor("out", [S, D], F32, kind="ExternalOutput").ap()
    skind = "ExternalOutput" if debug else "Internal"
    zT_d = nc.dram_tensor("zT_s", [1536, S], F32R, kind=skind).ap()
    v_d = nc.dram_tensor("v_s", [S, 512], F32R, kind=skind).ap()
    mix_d = nc.dram_tensor("mix_s", [D, S], F32R, kind=skind).ap()
    hs_d = nc.dram_tensor("hs_s", [S, HSW], F32, kind=skind).ap()
    acc_d = nc.dram_tensor("acc_s", [S, D], F32, kind=skind).ap()

    with ExitStack() as ctx:
        P = Prog(nc, ctx)
        COLS_R = 35600
        COLS_F = 15600
        bigR = ctx.enter_context(nc.sbuf_tensor("bigR", [128, COLS_R], F32R))
        bigF = ctx.enter_context(nc.sbuf_tensor("bigF", [128, COLS_F], F32))
        cur = {"R": 0, "F": 0}

        def allocR(n):
            a = cur["R"]
            cur["R"] += n
            assert cur["R"] <= COLS_R, ("R", cur["R"])
            return bigR[:, a:a + n]

        def allocF(n):
            a = cur["F"]
            cur["F"] += n
            assert cur["F"] <= COLS_F, ("F", cur["F"])
            return bigF[:, a:a + n]

        def mark():
            return dict(cur)

        def reset(m):
            cur.update(m)

        idxi_t = ctx.enter_context(nc.sbuf_tensor("idxi", [128, 4], I32))
        psb = [ctx.enter_context(nc.psum_tensor("ps%d" % i, [128, 512], F32)) for i in range(8)]
        PS = [Buf("ps%d" % i) for i in range(8)]

        ident = allocF(128)
        gcols = allocF(32)
        slotid = allocF(2)
        junkA = allocF(1024)
        junkD = allocF(512)
        B_const = Buf("const")
        B_gcols = Buf("gcols")
        B_junkA = Buf("junkA")
        B_junkD = Buf("junkD")
        P.op("sync", lambda e: e.dma_start(out=gcols, in_=gcols_d), w=[B_gcols], dma=True, sb=B_gcols)
        B_cb = Buf("cbuild")
        cb = allocF(640)
        aff_all = allocF(256)
        P.op("pool", lambda e: e.memset(ident, 1.0), w=[B_cb, B_const])
        P.op("pool", lambda e: e.affine_select(out=ident, in_=ident, pattern=[[-1, 128]], compare_op=ALU.is_equal,
                                               fill=0.0, base=0, channel_multiplier=1), w=[B_cb, B_const])
        P.op("pool", lambda e: e.iota(slotid, pattern=[[128, 2]], base=0, channel_multiplier=1,
                                      allow_small_or_imprecise_dtypes=True), w=[B_cb, B_const])
        P.op("pool", lambda e: e.memset(cb, 0.0), w=[B_cb])
        P.op("pool", lambda e: e.tensor_copy(out=cb[:, 0:128], in_=ident), w=[B_cb])
        P.op("pool", lambda e: e.memset(cb[0:64, 128:192], 1.0 / 64), w=[B_cb])
        P.op("pool", lambda e: e.memset(cb[64:128, 192:256], 1.0 / 64), w=[B_cb])
        P.op("pool", lambda e: e.memset(cb[:, 256:320], 1.0), w=[B_cb])
        P.op("pool", lambda e: e.memset(cb[:, 448:512], 1.0), w=[B_cb])
        P.op("pool", lambda e: e.memset(cb[:, 512:640], 1.0), w=[B_cb])
        crr = allocR(640)
        onesall = crr[:, 512:640]
        identr = crr[:, 0:128]
        blk64 = crr[:, 128:256]
        onesA = crr[:, 256:384]
        onesB = crr[:, 384:512]
        P.op("dve", lambda e: e.tensor_copy(out=crr.bitcast(F32R), in_=cb), r=[B_cb], w=[B_const])
        B_zT = Buf("zT"); B_v = Buf("v_d"); B_mix = Buf("mix_d"); B_acc = Buf("acc_d"); B_hs = Buf("hs_d"); B_out = Buf("out")
        DRAMB = [B_zT, B_v, B_mix, B_acc, B_hs, B_out]

        def finish():
            P.op("pool", None, r=DRAMB)
            P.op("sync", None, r=DRAMB)
            P.run()

        base_mark = mark()

        def rstd_from_ss(ss_col, out_col, bufs_r, bufs_w, n):
            P.op("act", lambda e: e.activation(out=out_col, in_=ss_col, func=AF.Ln, bias=EPS, scale=1.0 / n), r=bufs_r, w=bufs_w)
            P.op("act", lambda e: e.activation(out=out_col, in_=out_col, func=AF.Exp, scale=-0.5), r=bufs_w, w=bufs_w)

        evac_flip = [0]

        def evac_scale(out_ap, in_ap, scal_ap, r, w):
            evac_flip[0] ^= 1
            if evac_flip[0]:
                return P.op("act", lambda e: e.activation(out=out_ap, in_=in_ap, func=AF.Copy, scale=scal_ap), r=r, w=w, part=True)
            return P.op("dve", lambda e: e.tensor_scalar(out=out_ap, in0=in_ap, scalar1=scal_ap, scalar2=None, op0=ALU.mult), r=r, w=w, part=True)

        def evac_copy(out_ap, in_ap, r, w):
            evac_flip[0] ^= 1
            if evac_flip[0]:
                return P.op("act", lambda e: e.activation(out=out_ap, in_=in_ap, func=AF.Copy), r=r, w=w, part=True)
            return P.op("dve", lambda e: e.tensor_copy(out=out_ap, in_=in_ap), r=r, w=w, part=True)

        win = allocR(8 * 2048)
        win3 = win.rearrange("p (c n) -> p c n", c=8)
        B_win = [Buf("win%d" % i) for i in range(8)]
        xc = [allocF(1024) for _ in range(8)]
        B_xc = [Buf("xc%d" % i) for i in range(8)]
        stat = allocF(64)
        B_stat = [Buf("stat%d" % i) for i in range(16)]
        P.op("pool", lambda e: e.memset(stat, 0.0), w=B_stat)
        hTb = [allocR(8 * 512) for _ in range(2)]
        B_hT = [[Buf("hT%d_%d" % (s_, dc)) for dc in range(8)] for s_ in range(2)]
        stg = [allocR(512) for _ in range(4)]
        B_stg = [Buf("stg%d" % i) for i in range(4)]
        stg_i = [0]
        ps_i = [0]

        def next_ps(lo, hi):
            i = lo + (ps_i[0] % (hi - lo))
            ps_i[0] += 1
            return i

        def store(out_dram, stg_ap, stg_buf, bdram, eng="pool"):
            P.op(eng, lambda e: e.dma_start(out=out_dram, in_=stg_ap.bitcast(F32R)), r=[stg_buf], w=[bdram],
                 dma=True, sb=stg_buf)

        def s1_front_chunk(tb, j):
            xo = (tb % 2) * 4
            c = tb * 4 + j
            xj = xc[xo + j]
            P.op("sync", lambda e, c=c, xj=xj: e.dma_start(out=xj, in_=x_d[ts(c, 128), :]), w=[B_xc[xo + j]], dma=True, sb=B_xc[xo + j])
            P.op("act", lambda e, c=c, xj=xj: e.activation(out=junkA, in_=xj, func=AF.Square, accum_out=stat[:, c:c + 1]),
                 r=[B_xc[xo + j]], w=[B_junkA, B_stat[c]])
            rstd_from_ss(stat[:, c:c + 1], stat[:, 16 + c:17 + c], [B_stat[c]], [B_stat[c]], D)
            P.op("act", lambda e, c=c, xj=xj: e.activation(out=xj, in_=xj, func=AF.Copy, scale=stat[:, 16 + c:17 + c]),
                 r=[B_xc[xo + j], B_stat[c]], w=[B_xc[xo + j]])

        def s1_front(tb):
            for j in range(4):
                s1_front_chunk(tb, j)

        def s1_back(tb):
            hs_ = tb % 2
            hT3 = hTb[hs_].rearrange("p (c n) -> p c n", c=8)
            xo = (tb % 2) * 4
            for dc in range(8):
                pi = next_ps(0, 2)
                for j in range(4):
                    P.op("pe", lambda e, pi=pi, j=j, dc=dc, xj=xc[xo + j]: e.transpose(out=psb[pi][:, ts(j, 128)], in_=xj[:, ts(dc, 128)], identity=ident),
                         r=[B_xc[xo + j], B_const], w=[PS[pi]])
                evac_scale(hT3[:, dc, :].bitcast(F32R), psb[pi][:, :], gcols[:, dc:dc + 1], [PS[pi], B_gcols], [B_hT[hs_][dc]])
            for n in range(12):
                pi = next_ps(2, 8)
                for dc in range(8):
                    P.op("pe", lambda e, pi=pi, n=n, dc=dc, hT3=hT3: e.matmul(psb[pi][:, :], lhsT=win3[:, dc, ts(n, 128)].bitcast(F32R),
                                                                          rhs=hT3[:, dc, :].bitcast(F32R), start=(dc == 0), stop=(dc == 7)),
                         r=[B_win[dc], B_hT[hs_][dc]], w=[PS[pi]])
                si = stg_i[0] % 4
                stg_i[0] += 1
                evac_copy(stg[si].bitcast(F32R), psb[pi][:, :], [PS[pi]], [B_stg[si]])
                store(zT_d[ts(n, 128), ts(tb, 512)], stg[si], B_stg[si], B_zT)
                if tb + 1 < 4 and n % 3 == 2:
                    s1_front_chunk(tb + 1, n // 3)
            for j in range(4):
                c = tb * 4 + j
                pi = next_ps(2, 8)
                for dc in range(8):
                    P.op("pe", lambda e, pi=pi, j=j, dc=dc, hT3=hT3: e.matmul(psb[pi][:, :], lhsT=hT3[:, dc, ts(j, 128)].bitcast(F32R),
                                                                          rhs=win3[:, dc, 1536:2048].bitcast(F32R), start=(dc == 0), stop=(dc == 7)),
                         r=[B_win[dc], B_hT[hs_][dc]], w=[PS[pi]])
                si = stg_i[0] % 4
                stg_i[0] += 1
                evac_copy(stg[si].bitcast(F32R), psb[pi][:, :], [PS[pi]], [B_stg[si]])
                store(v_d[ts(c, 128), :], stg[si], B_stg[si], B_v)

        s1_front(0)
        for dc in range(8):
            P.op("sync", lambda e, dc=dc: e.dma_start(out=win3[:, dc, :].bitcast(F32R), in_=win_d[ts(dc, 128), :]),
                 w=[B_win[dc]], dma=True, sb=B_win[dc])
        for tb in range(4):
            s1_back(tb)

        if stop == 1:
            finish()
            return nc
        reset(base_mark)
        stg = [allocR(512) for _ in range(4)]
        B_region = Buf("region")

        def region_barrier(bufs):
            P.barrier(bufs)

        all_s1 = B_win + [B_zT, B_v] + B_xc + B_stat + B_hT[0] + B_hT[1] + B_stg + PS + [B_junkA]
        region_barrier(all_s1)
        B_stg = [Buf("stg2_%d" % i) for i in range(4)]
        upad = [allocF(2064) for _ in range(2)]
        B_up = [Buf("upad%d" % i) for i in range(2)]
        ta = allocF(2064)
        tb_ = allocF(2064)
        rc = allocF(2048)
        dn = allocR(2048)
        wpool = allocR(4 * 128)
        wpool3 = wpool.rearrange("p (g n) -> p g n", g=4)
        B_ta = Buf("ta")
        B_tb = Buf("tb")
        B_rc = Buf("rc")
        B_dn = Buf("dn")
        B_wpool = Buf("wpool")
        P.op("sync", lambda e: e.dma_start(out=wpool3.bitcast(F32R), in_=wpool_d.rearrange("g c n -> c g n")), w=[B_wpool], dma=True, sb=B_wpool)
        for i in range(2):
            P.op("pool", lambda e, i=i: e.memset(upad[i], 0.0), w=[B_up[i]])
        def emit_pool_dve(g):
            wsz = (2, 4, 8, 16)[g]
            u = upad[g % 2]
            bu = B_up[g % 2]
            P.op("sync", lambda e, g=g, u=u: e.dma_start(out=u[:, 8:2056].bitcast(F32R), in_=zT_d[ts(g, 128), :]), r=[B_zT], w=[bu], dma=True, sb=bu)
            hw = wsz // 2
            P.op("pool", lambda e, wsz=wsz: e.memset(rc, 1.0 / wsz), w=[B_rc])
            for t in range(hw):
                P.op("pool", lambda e, t=t, hw=hw: e.memset(rc[:, t:t + 1], 1.0 / (t + hw)), w=[B_rc])
            for t in range(S - hw + 1, S):
                P.op("pool", lambda e, t=t, hw=hw: e.memset(rc[:, t:t + 1], 1.0 / (S - t + hw)), w=[B_rc])
            P.op("pool", lambda e, u=u: e.tensor_tensor(out=ta[:, 0:2063], in0=u[:, 0:2063], in1=u[:, 1:2064], op=ALU.add), r=[bu], w=[B_ta])
            src, bsrc, width = ta, B_ta, 2063
            other, bother = tb_, B_tb
            step = 2
            while step < wsz:
                nw = width - step
                P.op("pool", lambda e, src=src, other=other, nw=nw, step=step: e.tensor_tensor(out=other[:, 0:nw], in0=src[:, 0:nw], in1=src[:, step:step + nw], op=ALU.add),
                     r=[bsrc], w=[bother])
                src, bsrc, other, bother = other, bother, src, bsrc
                width = nw
                step *= 2
            off = 8 - hw
            P.op("pool", lambda e, src=src, other=other, off=off: e.tensor_tensor(out=other[:, 0:2048], in0=src[:, off:off + 2048], in1=rc, op=ALU.mult),
                 r=[bsrc, B_rc], w=[bother])
            P.op("pool", lambda e, other=other, u=u: e.tensor_tensor(out=dn.bitcast(F32R), in0=other[:, 0:2048], in1=u[:, 8:2056], op=ALU.subtract),
                 r=[bother, bu], w=[B_dn])

        def emit_pool_mm(g):
            for tb in range(4):
                pi = 7
                P.op("pe", lambda e, pi=pi, g=g, tb=tb: e.matmul(psb[pi][:, :], lhsT=wpool3[:, g, :].bitcast(F32R), rhs=dn[:, ts(tb, 512)].bitcast(F32R), start=True, stop=True),
                     r=[B_wpool, B_dn], w=[PS[pi]])
                si = stg_i[0] % 4
                stg_i[0] += 1
                evac_scale(stg[si].bitcast(F32R), psb[pi][:, :], gcols[:, 24 + g:25 + g], [PS[pi], B_gcols], [B_stg[si]])
                store(mix_d[ts(g, 128), ts(tb, 512)], stg[si], B_stg[si], B_mix)

        if stop == 2:
            for g in range(4):
                emit_pool_dve(g)
                emit_pool_mm(g)
            finish()
            return nc
        qraw = [allocR(2048)] * 2
        kraw = [allocR(2048)] * 2
        B_qraw = [Buf("qraw")] * 2
        B_kraw = [Buf("kraw")] * 2
        sq = allocR(2048)
        B_sq = Buf("sq")
        rsd = allocF(512)
        B_rsd = Buf("rsd")
        qn = [allocR(2048) for _ in range(2)]
        knA = [allocR(2048) for _ in range(2)]
        knB = [allocR(2048) for _ in range(2)]
        B_qn = [Buf("qn%d" % i) for i in range(2)]
        B_kn = [Buf("kn%d" % i) for i in range(2)]
        vA = allocR(16 * 128)
        vB = allocR(16 * 128)
        vA3 = vA.rearrange("p (c n) -> p c n", c=16)
        vB3 = vB.rearrange("p (c n) -> p c n", c=16)
        B_vA = Buf("vA")
        B_vB = Buf("vB")
        NBT = 8
        bt = [allocR(512) for _ in range(NBT)]
        B_bt = [Buf("bt%d" % i) for i in range(NBT)]
        Pt = [allocR(512) for _ in range(3)]
        B_Pt = [Buf("Pt%d" % i) for i in range(3)]
        rD = allocF(512)
        B_rD = Buf("rD")
        tmpS = [allocF(512) for _ in range(3)]
        B_tmpS = [Buf("tmpS%d" % i) for i in range(3)]
        for q4 in range(4):
            for i2 in range(2):
                P.op("dve", lambda e, q4=q4, i2=i2: e.tensor_scalar(out=knA[i2][64:128, ts(q4, 512)], in0=cb[64:128, 0:512], scalar1=0.0, scalar2=None, op0=ALU.mult), r=[B_cb], w=[B_kn[i2]])
                P.op("dve", lambda e, q4=q4, i2=i2: e.tensor_scalar(out=knB[i2][0:64, ts(q4, 512)], in0=cb[0:64, 0:512], scalar1=0.0, scalar2=None, op0=ALU.mult), r=[B_cb], w=[B_kn[i2]])
        for q4 in range(4):
            P.op("dve", lambda e, q4=q4: e.tensor_scalar(out=vA[:, ts(q4, 512)], in0=cb[:, 0:512], scalar1=0.0, scalar2=None, op0=ALU.mult), r=[B_cb], w=[B_vA])
            P.op("dve", lambda e, q4=q4: e.tensor_scalar(out=vB[:, ts(q4, 512)], in0=cb[:, 0:512], scalar1=0.0, scalar2=None, op0=ALU.mult), r=[B_cb], w=[B_vB])
        v_d3 = v_d.rearrange("(c p) n -> p c n", p=128)
        bt_i = [0]
        pt_i = [0]
        for pr in range(4):
            sl = pr % 2
            P.op("sync", lambda e, pr=pr, sl=sl: e.dma_start(out=qraw[sl].bitcast(F32R), in_=zT_d[512 + pr * 128:512 + (pr + 1) * 128, :]), r=[B_zT], w=[B_qraw[sl]], dma=True, sb=B_qraw[sl])
            P.op("sync", lambda e, pr=pr, sl=sl: e.dma_start(out=kraw[sl].bitcast(F32R), in_=zT_d[1024 + pr * 128:1024 + (pr + 1) * 128, :]), r=[B_zT], w=[B_kraw[sl]], dma=True, sb=B_kraw[sl])
            P.op("sync", lambda e, pr=pr: e.dma_start(out=vA3[:, :, 0:64].bitcast(F32R), in_=v_d3[:, :, pr * 128:pr * 128 + 64]), r=[B_v], w=[B_vA], dma=True, sb=B_vA)
            P.op("sync", lambda e, pr=pr: e.dma_start(out=vB3[:, :, 64:128].bitcast(F32R), in_=v_d3[:, :, pr * 128 + 64:pr * 128 + 128]), r=[B_v], w=[B_vB], dma=True, sb=B_vB)
            for which in range(2):
                raw = (qraw, kraw)[which][sl]
                braw = (B_qraw, B_kraw)[which][sl]
                dst = qn[sl]
                bdst = (B_qn, B_kn)[which][sl]
                gcol = gcols[:, 28 + which:29 + which]
                P.op("act", lambda e, raw=raw: e.activation(out=sq.bitcast(F32R), in_=raw.bitcast(F32), func=AF.Square), r=[braw], w=[B_sq])
                for tb in range(4):
                    pi = next_ps(7, 8)
                    P.op("pe", lambda e, pi=pi, tb=tb: e.matmul(psb[pi][:, :], lhsT=blk64.bitcast(F32R), rhs=sq[:, ts(tb, 512)].bitcast(F32R), start=True, stop=True),
                         r=[B_const, B_sq], w=[PS[pi]])
                    sc_ = 64.0 if which == 0 else 1.0
                    P.op("act", lambda e, pi=pi, sc_=sc_: e.activation(out=rsd, in_=psb[pi][:, :], func=AF.Ln, bias=EPS * sc_, scale=sc_), r=[PS[pi]], w=[B_rsd])
                    P.op("act", lambda e: e.activation(out=rsd, in_=rsd, func=AF.Exp, scale=-0.5), r=[B_rsd], w=[B_rsd])
                    if which == 0:
                        P.op("dve", lambda e, raw=raw, dst=dst, tb=tb, gcol=gcol: e.scalar_tensor_tensor(out=dst[:, ts(tb, 512)].bitcast(F32R), in0=raw[:, ts(tb, 512)].bitcast(F32), scalar=gcol,
                                                                                                     in1=rsd, op0=ALU.mult, op1=ALU.mult),
                             r=[braw, B_rsd, B_gcols], w=[bdst])
                    else:
                        for (lo, hi, kdst) in ((0, 64, knA[sl]), (64, 128, knB[sl])):
                            P.op("dve", lambda e, raw=raw, kdst=kdst, tb=tb, lo=lo, hi=hi: e.scalar_tensor_tensor(out=kdst[lo:hi, ts(tb, 512)].bitcast(F32R), in0=raw[lo:hi, ts(tb, 512)].bitcast(F32),
                                                                                                          scalar=gcols[lo:hi, 29:30], in1=rsd[lo:hi, :], op0=ALU.mult, op1=ALU.mult),
                                 r=[braw, B_rsd, B_gcols], w=[bdst])
            emit_pool_dve(pr)
            steps = [(qb, hh, j) for qb in range(4) for hh in range(2) for j in QB_CHUNKS[qb]]
            nst = len(steps)
            bslot = {}
            qk_info = {}

            def emit_bdma(si_):
                qb, hh, j = steps[si_]
                h = pr * 2 + hh
                bi = bt_i[0] % NBT
                bt_i[0] += 1
                bslot[si_] = bi
                P.op("sync", lambda e, h=h, qb=qb, j=j, bi=bi: e.dma_start(out=bt[bi].bitcast(F32R), in_=bias_d[h, bias_tile_id(qb, j), :, :]), w=[B_bt[bi]], dma=True, sb=B_bt[bi])

            def emit_qk(si_):
                qb, hh, j = steps[si_]
                pi = next_ps(0, 3)
                bi = bslot[si_]
                hb = 64 * hh
                kpad = knA[sl] if hh == 0 else knB[sl]
                P.op("pe", lambda e, pi=pi, kpad=kpad, j=j, qb=qb, sl=sl: e.matmul(psb[pi][:, :], lhsT=kpad[:, ts(j, 128)].bitcast(F32R),
                                                                             rhs=qn[sl][:, ts(qb, 512)].bitcast(F32R), start=True, stop=True),
                     r=[B_kn[sl], B_qn[sl]], w=[PS[pi]])
                ti = pt_i[0] % 3
                pt_i[0] += 1
                P.op("dve", lambda e, pi=pi, bi=bi, ti=ti: e.tensor_tensor(out=tmpS[ti], in0=psb[pi][:, :], in1=bt[bi].bitcast(F32), op=ALU.add),
                     r=[PS[pi], B_bt[bi]], w=[B_tmpS[ti]])
                P.op("act", lambda e, ti=ti: e.activation(out=Pt[ti].bitcast(F32R), in_=tmpS[ti], func=AF.Exp), r=[B_tmpS[ti]], w=[B_Pt[ti]])
                qk_info[si_] = ti

            def emit_pv(si_):
                qb, hh, j = steps[si_]
                nps, dps = (3, 4) if (qb % 2 == 0) else (5, 6)
                ti = qk_info[si_]
                first = (si_ == 0) or (steps[si_ - 1][0] != qb)
                last = (si_ == nst - 1) or (steps[si_ + 1][0] != qb)
                vt3 = vA3 if hh == 0 else vB3
                bv = B_vA if hh == 0 else B_vB
                on = onesA if hh == 0 else onesB
                extra = []
                P.op("pe", lambda e, vt3=vt3, j=j, ti=ti, first=first, last=last, nps=nps: e.matmul(psb[nps][:, :], lhsT=vt3[:, j, :].bitcast(F32R), rhs=Pt[ti].bitcast(F32R), start=first, stop=last),
                     r=[bv, B_Pt[ti]] + extra, w=[PS[nps]])
                P.op("pe", lambda e, on=on, ti=ti, first=first, last=last, dps=dps: e.matmul(psb[dps][:, :], lhsT=on.bitcast(F32R), rhs=Pt[ti].bitcast(F32R), start=first, stop=last),
                     r=[B_const, B_Pt[ti]], w=[PS[dps]])
                if last:
                    P.op("act", lambda e, dps=dps: e.activation(out=rD, in_=psb[dps][:, :], func=AF.Ln), r=[PS[dps]], w=[B_rD])
                    P.op("act", lambda e: e.activation(out=rD, in_=rD, func=AF.Exp, scale=-1.0), r=[B_rD], w=[B_rD])
                    si = stg_i[0] % 4
                    stg_i[0] += 1
                    P.op("dve", lambda e, nps=nps, si=si: e.tensor_tensor(out=stg[si].bitcast(F32R), in0=psb[nps][:, :], in1=rD, op=ALU.mult), r=[PS[nps], B_rD], w=[B_stg[si]])
                    store(mix_d[512 + pr * 128:512 + (pr + 1) * 128, ts(qb, 512)], stg[si], B_stg[si], B_mix)

            for si_ in range(min(NBT - 1, nst)):
                emit_bdma(si_)
            emit_qk(0)
            if nst > 1:
                emit_qk(1)
            for si_ in range(nst):
                if si_ + NBT - 1 < nst:
                    emit_bdma(si_ + NBT - 1)
                if si_ + 2 < nst:
                    emit_qk(si_ + 2)
                emit_pv(si_)
            emit_pool_mm(pr)

        if stop == 3:
            finish()
            return nc
        all_s3 = (B_tmpS + [B_zT, B_v, B_mix, B_sq, B_rsd, B_vA, B_vB, B_rD, B_ta, B_tb, B_rc, B_dn, B_wpool] + B_up + B_qraw + B_kraw + B_qn + B_kn + B_bt + B_Pt + B_stg + PS)
        region_barrier(all_s3)
        reset(base_mark)
        wo = allocR(8 * 1024)
        wo3 = wo.rearrange("p (c n) -> p c n", c=8)
        B_wo = Buf("wo")
        P.op("sync", lambda e: e.dma_start(out=wo3.bitcast(F32R), in_=wout_d.rearrange("(c p) n -> p c n", p=128)), w=[B_wo], dma=True, sb=B_wo)
        wr = allocR(8 * 16)
        wr3 = wr.rearrange("p (c n) -> p c n", c=8)
        B_wr = Buf("wr")
        P.op("sync", lambda e: e.dma_start(out=wr3.bitcast(F32R), in_=wr_d.rearrange("(c p) n -> p c n", p=128)), w=[B_wr], dma=True, sb=B_wr)
        gffn_rep = allocF(1024)
        B_gfr = Buf("gffn_rep")
        P.op("sync", lambda e: e.dma_start(out=gffn_rep, in_=grep_d[0]), w=[B_gfr], dma=True, sb=B_gfr)
        mT = [allocR(8 * 128) for _ in range(2)]
        B_mT = [Buf("mT%d" % i) for i in range(2)]
        xc = [allocF(1024) for _ in range(2)]
        B_xc = [Buf("xc4_%d" % i) for i in range(2)]
        x1c = [allocF(1024) for _ in range(2)]
        B_x1 = [Buf("x1c%d" % i) for i in range(2)]
        xs2 = [allocF(HSW) for _ in range(3)]
        B_xs2 = [Buf("xs2_%d" % i) for i in range(3)]
        h2T = [allocR(8 * 128) for _ in range(2)]
        B_h2T = [Buf("h2T%d" % i) for i in range(2)]
        aff3 = aff_all.rearrange("p (c n) -> p c n", c=16)
        B_aff = [Buf("aff%d" % i) for i in range(16)]
        st4 = allocF(64)
        B_st4 = [Buf("st4_%d" % i) for i in range(16)]
        P.op("pool", lambda e: e.memset(st4, 0.0), w=B_st4)
        mix_d3 = mix_d.rearrange("(c p) t -> p c t", p=128)
        def s4_front(c):
            s2 = c % 2
            mT3 = mT[s2].rearrange("p (c n) -> p c n", c=8)
            P.op("sync", lambda e, c=c, mT3=mT3: e.dma_start(out=mT3.bitcast(F32R), in_=mix_d3[:, :, ts(c, 128)]), r=[B_mix], w=[B_mT[s2]], dma=True, sb=B_mT[s2])
            P.op("sync", lambda e, c=c, s2=s2: e.dma_start(out=xc[s2], in_=x_d[ts(c, 128), :]), w=[B_xc[s2]], dma=True, sb=B_xc[s2])
            for half in range(2):
                pi = half
                for k in range(8):
                    P.op("pe", lambda e, pi=pi, k=k, half=half, mT3=mT3: e.matmul(psb[pi][:, :], lhsT=mT3[:, k, :].bitcast(F32R), rhs=wo3[:, k, ts(half, 512)].bitcast(F32R),
                                                                            start=(k == 0), stop=(k == 7)),
                         r=[B_mT[s2], B_wo], w=[PS[pi]])
                P.op("dve", lambda e, pi=pi, half=half, s2=s2: e.tensor_tensor(out=x1c[s2][:, ts(half, 512)], in0=psb[pi][:, :], in1=xc[s2][:, ts(half, 512)], op=ALU.add),
                     r=[PS[pi], B_xc[s2]], w=[B_x1[s2]])
            P.op("pool", lambda e, c=c, s2=s2: e.dma_start(out=acc_d[ts(c, 128), :], in_=x1c[s2]), r=[B_x1[s2]], w=[B_acc], dma=True, sb=B_x1[s2])
            P.op("act", lambda e, c=c, s2=s2: e.activation(out=junkA, in_=x1c[s2], func=AF.Square, accum_out=st4[:, c:c + 1]), r=[B_x1[s2]], w=[B_junkA, B_st4[c]])
            rstd_from_ss(st4[:, c:c + 1], st4[:, 16 + c:17 + c], [B_st4[c]], [B_st4[c]], D)
            P.op("dve", lambda e, c=c, s2=s2: e.scalar_tensor_tensor(out=xs2[c % 3][:, 0:D], in0=x1c[s2], scalar=st4[:, 16 + c:17 + c], in1=gffn_rep, op0=ALU.mult, op1=ALU.mult),
                 r=[B_x1[s2], B_st4[c], B_gfr], w=[B_xs2[c % 3]])
            pA = 4 + 2 * s2
            pB = 5 + 2 * s2
            for dc in range(8):
                pi = pA if dc < 4 else pB
                P.op("pe", lambda e, pi=pi, dc=dc, s2=s2: e.transpose(out=psb[pi][:, ts(dc % 4, 128)], in_=xs2[c % 3][:, ts(dc, 128)], identity=ident),
                     r=[B_xs2[c % 3], B_const], w=[PS[pi]])
            evac_copy(h2T[s2][:, 0:512], psb[pA][:, :], [PS[pA]], [B_h2T[s2]])
            evac_copy(h2T[s2][:, 512:1024], psb[pB][:, :], [PS[pB]], [B_h2T[s2]])

        def s4_back(c):
            s2 = c % 2
            h2T3 = h2T[s2].rearrange("p (c n) -> p c n", c=8)
            pr_ = 2 + s2
            for dc in range(8):
                P.op("pe", lambda e, pr_=pr_, dc=dc, h2T3=h2T3: e.matmul(psb[pr_][:, 0:16], lhsT=h2T3[:, dc, :].bitcast(F32R), rhs=wr3[:, dc, :].bitcast(F32R), start=(dc == 0), stop=(dc == 7)),
                     r=[B_h2T[s2], B_wr], w=[PS[pr_]])
            P.op("dve", lambda e, pr_=pr_, c=c: e.tensor_reduce(out=st4[:, 32 + c:33 + c], in_=psb[pr_][:, 0:16], axis=AX.X, op=ALU.max), r=[PS[pr_]], w=[B_st4[c]])
            P.op("dve", lambda e, c=c: e.tensor_scalar(out=st4[:, 32 + c:33 + c], in0=st4[:, 32 + c:33 + c], scalar1=-1.0, scalar2=None, op0=ALU.mult), r=[B_st4[c]], w=[B_st4[c]])
            P.op("act", lambda e, pr_=pr_, c=c: e.activation(out=aff3[:, c, :], in_=psb[pr_][:, 0:16], func=AF.Exp, bias=st4[:, 32 + c:33 + c], scale=1.0, accum_out=st4[:, 48 + c:49 + c]),
                 r=[PS[pr_], B_st4[c]], w=[B_aff[c], B_st4[c]])
            P.op("dve", lambda e, c=c: e.reciprocal(out=st4[:, 48 + c:49 + c], in_=st4[:, 48 + c:49 + c]), r=[B_st4[c]], w=[B_st4[c]])
            P.op("dve", lambda e, c=c: e.tensor_scalar(out=aff3[:, c, :], in0=aff3[:, c, :], scalar1=st4[:, 48 + c:49 + c], scalar2=None, op0=ALU.mult), r=[B_st4[c], B_aff[c]], w=[B_aff[c]])
            P.op("dve", lambda e, c=c, s2=s2: e.tensor_copy(out=xs2[c % 3][:, D:HSW], in_=aff3[:, c, :]), r=[B_aff[c]], w=[B_xs2[c % 3]])
            P.op("pool", lambda e, c=c, s2=s2: e.dma_start(out=hs_d[ts(c, 128), :], in_=xs2[c % 3]), r=[B_xs2[c % 3]], w=[B_hs], dma=True, sb=B_xs2[c % 3])

        s4_front(0)
        for c in range(NT):
            if c + 1 < NT:
                s4_front(c + 1)
            s4_back(c)

        if stop == 4:
            finish()
            return nc
        all_s4 = [B_wo, B_wr, B_mix, B_junkA] + B_mT + B_xc + B_x1 + B_xs2 + B_h2T + B_st4 + PS
        region_barrier(all_s4)
        reset(base_mark)
        xe_all = allocF(2 * 2 * HSW)
        affT = xe_all[:, 0:2048]
        work = xe_all[:, 2048:4096]
        cum = allocR(2048)
        cume = allocR(2048)
        B_cume = Buf("cume")
        mx8 = allocF(8)
        B_affT = Buf("affT")
        B_work = Buf("work")
        B_cum = Buf("cum")
        for tb in range(4):
            pi = next_ps(0, 4)
            for j in range(4):
                c = tb * 4 + j
                P.op("pe", lambda e, pi=pi, j=j, c=c: e.matmul(psb[pi][0:16, ts(j, 128)], lhsT=aff3[:, c, :], rhs=ident, start=True, stop=True),
                     r=[B_aff[c], B_const], w=[PS[pi]])
            P.op("dve", lambda e, pi=pi, tb=tb: e.tensor_copy(out=affT[0:16, ts(tb, 512)], in_=psb[pi][0:16, :]), r=[PS[pi]], w=[B_affT])
        for rnd in range(CAP // 8):
            srcw = affT if rnd == 0 else work
            P.op("dve", lambda e, srcw=srcw: e.max(out=mx8[0:16, :], in_=srcw[0:16, :]), r=[B_affT, B_work], w=[B_work])
            if rnd < CAP // 8 - 1:
                P.op("dve", lambda e, srcw=srcw: e.match_replace(out=work[0:16, :], in_to_replace=mx8[0:16, :], in_values=srcw[0:16, :], imm_value=-1.0),
                     r=[B_affT, B_work], w=[B_work])
        P.op("dve", lambda e: e.tensor_scalar(out=work[0:16, :], in0=affT[0:16, :], scalar1=mx8[0:16, 7:8], scalar2=None, op0=ALU.is_ge), r=[B_affT, B_work], w=[B_work])
        P.op("dve", lambda e: e.tensor_tensor_scan(out=cum[0:16, :].bitcast(F32R), data0=work[0:16, :], data1=work[0:16, :], initial=0.0, op0=ALU.add, op1=ALU.max),
             r=[B_work], w=[B_cum])

        moe_mark = mark()
        NW = 6
        wring = [allocR(4096) for _ in range(NW)]
        B_wr_ = [Buf("wring%d" % i) for i in range(NW)]
        xe = [xe_all[:, 0:2 * HSW], xe_all[:, 2 * HSW:4 * HSW]]
        B_xe = [Buf("xe%d" % i) for i in range(2)]
        xeT = [allocR(8 * 256)] * 2
        B_xeT = [Buf("xeT")] * 2
        actb = allocR(16 * 256)
        act3 = actb.rearrange("p (c n) -> p c n", c=16)
        B_act = [Buf("act%d" % i) for i in range(16)]
        sg = [allocF(256) for _ in range(2)]
        B_sg = [Buf("sg%d" % i) for i in range(2)]
        ye = [allocF(2 * 1024) for _ in range(2)]
        B_ye = [Buf("ye%d" % i) for i in range(2)]
        cnt = allocF(16)
        idxf = allocF(4)
        B_idx = [Buf("idx%d" % i) for i in range(2)]
        B_cnt = Buf("cnt")
        wi = [0]

        def wload(dram_ap, shape_c):
            i = wi[0] % NW
            wi[0] += 1
            view = wring[i].rearrange("p (c n) -> p c n", c=shape_c)
            P.op("sync", lambda e, view=view, dram_ap=dram_ap: e.dma_start(out=view.bitcast(F32R), in_=dram_ap), w=[B_wr_[i]], dma=True, sb=B_wr_[i])
            return view, B_wr_[i]

        def emit_idx(ex):
            s_ = ex % 2
            P.op("dve", lambda e, ex=ex: e.tensor_scalar(out=cume[0:16, :], in0=cum[0:16, :].bitcast(F32), scalar1=ident[0:16, ex:ex + 1], scalar2=None, op0=ALU.mult),
                 r=[B_cum, B_const], w=[B_cume])
            for tb in range(4):
                pi = next_ps(0, 2)
                P.op("pe", lambda e, pi=pi, tb=tb: e.matmul(psb[pi][:, :], lhsT=onesall[0:16, :], rhs=cume[0:16, ts(tb, 512)], start=True, stop=True),
                     r=[B_const, B_cume], w=[PS[pi]])
                for sc in range(2):
                    P.op("dve", lambda e, pi=pi, tb=tb, sc=sc: e.tensor_scalar(out=junkD, in0=psb[pi][:, :], scalar1=slotid[:, sc:sc + 1], scalar2=0.0, op0=ALU.is_le, op1=ALU.add,
                                                                         accum_out=cnt[:, sc * 4 + tb:sc * 4 + tb + 1]),
                         r=[PS[pi], B_const], w=[B_junkD, B_cnt])
            P.op("dve", lambda e: e.tensor_reduce(out=idxf[:, 0:2], in_=cnt[:, 0:8].rearrange("p (s t) -> p s t", s=2), axis=AX.X, op=ALU.add), r=[B_cnt], w=[B_cnt])
            P.op("dve", lambda e: e.tensor_scalar(out=idxf[:, 0:2], in0=idxf[:, 0:2], scalar1=float(S - 1), scalar2=None, op0=ALU.min), r=[B_cnt], w=[B_cnt])
            P.op("dve", lambda e, s_=s_: e.tensor_copy(out=idxi_t[:, 2 * s_:2 * s_ + 2], in_=idxf[:, 0:2]), r=[B_cnt], w=[B_idx[s_]])
            xe3 = xe[s_].rearrange("p (s n) -> p s n", s=2)
            for sc in range(2):
                P.op("pool", lambda e, s_=s_, sc=sc, xe3=xe3: e.indirect_dma_start(out=xe3[:, sc, :], out_offset=None, in_=hs_d,
                                                                               in_offset=bass.IndirectOffsetOnAxis(ap=idxi_t[:, 2 * s_ + sc:2 * s_ + sc + 1], axis=0)),
                     r=[B_idx[s_], B_hs], w=[B_xe[s_]], dma=True, sb=B_xe[s_])

        def emit_transposes(ex):
            s_ = ex % 2
            xe3 = xe[s_].rearrange("p (s n) -> p s n", s=2)
            for b4 in range(4):
                pi = next_ps(0, 2)
                for dd in range(2):
                    dc = b4 * 2 + dd
                    for sc in range(2):
                        P.op("pe", lambda e, pi=pi, dd=dd, sc=sc, dc=dc, xe3=xe3: e.transpose(out=psb[pi][:, (dd * 2 + sc) * 128:(dd * 2 + sc + 1) * 128], in_=xe3[:, sc, ts(dc, 128)], identity=ident),
                             r=[B_xe[s_], B_const], w=[PS[pi]])
                evac_copy(xeT[s_][:, b4 * 512:(b4 + 1) * 512], psb[pi][:, :], [PS[pi]], [B_xeT[s_]])

        def emit_gateup(ex):
            s_ = ex % 2
            xeT3 = xeT[s_].rearrange("p (c n) -> p c n", c=8)
            for fb in range(4):
                wgv, bwg = wload(wg_d[ex].rearrange("(c p) n -> p c n", p=128)[:, :, ts(fb, 512)], 8)
                wuv, bwu = wload(wu_d[ex].rearrange("(c p) n -> p c n", p=128)[:, :, ts(fb, 512)], 8)
                for fi in range(4):
                    fc = fb * 4 + fi
                    pi = next_ps(2, 4)
                    for dc in range(8):
                        P.op("pe", lambda e, pi=pi, wgv=wgv, fi=fi, dc=dc, xeT3=xeT3: e.matmul(psb[pi][:, 0:256], lhsT=wgv[:, dc, ts(fi, 128)].bitcast(F32R), rhs=xeT3[:, dc, :].bitcast(F32R),
                                                                                        start=(dc == 0), stop=(dc == 7)),
                             r=[bwg, B_xeT[s_]], w=[PS[pi]])
                    for dc in range(8):
                        P.op("pe", lambda e, pi=pi, wuv=wuv, fi=fi, dc=dc, xeT3=xeT3: e.matmul(psb[pi][:, 256:512], lhsT=wuv[:, dc, ts(fi, 128)].bitcast(F32R), rhs=xeT3[:, dc, :].bitcast(F32R),
                                                                                        start=(dc == 0), stop=(dc == 7)),
                             r=[bwu, B_xeT[s_]], w=[PS[pi]])
                    gi = fc % 2
                    P.op("act", lambda e, pi=pi, gi=gi: e.activation(out=sg[gi], in_=psb[pi][:, 0:256], func=AF.Silu), r=[PS[pi]], w=[B_sg[gi]])
                    P.op("dve", lambda e, pi=pi, gi=gi, fc=fc: e.tensor_tensor(out=act3[:, fc, :].bitcast(F32R), in0=sg[gi], in1=psb[pi][:, 256:512], op=ALU.mult),
                         r=[PS[pi], B_sg[gi]], w=[B_act[fc]])

        def emit_down(ex):
            s_ = ex % 2
            xe3 = xe[s_].rearrange("p (s n) -> p s n", s=2)
            for fb in range(4):
                wdv, bwd = wload(wd_d[ex].rearrange("(c p) n -> p c n", p=128)[:, fb * 4:(fb + 1) * 4, :], 4)
                for fi in range(4):
                    fc = fb * 4 + fi
                    for sc in range(2):
                        for half in range(2):
                            pi = 4 + sc * 2 + half
                            P.op("pe", lambda e, pi=pi, fc=fc, sc=sc, half=half, fi=fi, wdv=wdv: e.matmul(psb[pi][:, :], lhsT=act3[:, fc, ts(sc, 128)].bitcast(F32R), rhs=wdv[:, fi, ts(half, 512)].bitcast(F32R),
                                                                                                  start=(fc == 0), stop=(fc == 15)),
                                 r=[B_act[fc], bwd], w=[PS[pi]])
            ye3 = ye[s_].rearrange("p (s n) -> p s n", s=2)
            for sc in range(2):
                for half in range(2):
                    pi = 4 + sc * 2 + half
                    evac_scale(ye3[:, sc, ts(half, 512)], psb[pi][:, :], xe3[:, sc, D + ex:D + ex + 1], [PS[pi], B_xe[s_]], [B_ye[s_]])
            for sc in range(2):
                P.op("pool", lambda e, s_=s_, sc=sc, ye3=ye3: e.indirect_dma_start(out=acc_d, out_offset=bass.IndirectOffsetOnAxis(ap=idxi_t[:, 2 * s_ + sc:2 * s_ + sc + 1], axis=0),
                                                                               in_=ye3[:, sc, :], in_offset=None, compute_op=ALU.add, bounds_check=S - 1, oob_is_err=True),
                     r=[B_ye[s_], B_idx[s_]], w=[B_acc], dma=True, sb=B_ye[s_], waw=True)

        emit_idx(0)
        emit_idx(1)
        emit_transposes(0)
        for ex in range(NE):
            emit_gateup(ex)
            if ex + 1 < NE:
                emit_transposes(ex + 1)
            emit_down(ex)
            if ex + 2 < NE:
                emit_idx(ex + 2)

        if stop == 6:
            finish()
            return nc
        all_s6 = [B_acc, B_hs, B_cum, B_cume, B_cnt, B_affT, B_work] + B_aff + B_wr_ + B_xe + B_xeT + B_act + B_sg + B_ye + B_idx + PS + [B_wo, B_wr] + B_mT + B_xc + B_x1 + B_xs2 + B_h2T + B_st4 + [B_junkA, B_junkD]
        region_barrier(all_s6)
        reset(base_mark)
        wpg = allocR(8 * 1024)
        wpg3 = wpg.rearrange("p (c n) -> p c n", c=8)
        wpp = allocR(2 * 1024)
        wpp3 = wpp.rearrange("p (c n) -> p c n", c=2)
        gpost = allocF(1024)
        gple_rep = allocF(1024)
        B_gpr = Buf("gple_rep")
        P.op("sync", lambda e: e.dma_start(out=gple_rep, in_=grep_d[1]), w=[B_gpr], dma=True, sb=B_gpr)
        B_wpg = Buf("wpg")
        P.op("sync", lambda e: e.dma_start(out=wpg3.bitcast(F32R), in_=wpg_d.rearrange("(c p) n -> p c n", p=128)), w=[B_wpg], dma=True, sb=B_wpg)
        B_wpp = Buf("wpp")
        P.op("sync", lambda e: e.dma_start(out=wpp3.bitcast(F32R), in_=wpp_d.rearrange("(c p) n -> p c n", p=128)), w=[B_wpp], dma=True, sb=B_wpp)
        B_gpost = Buf("gpost")
        P.op("sync", lambda e: e.dma_start(out=gpost, in_=gpost_d), w=[B_gpost], dma=True, sb=B_gpost)
        x2c = [allocF(1024) for _ in range(3)]
        B_x2 = [Buf("x2c%d" % i) for i in range(3)]
        xs3 = [allocF(1024) for _ in range(2)]
        B_xs3 = [Buf("xs3_%d" % i) for i in range(2)]
        h3T = [allocR(8 * 128) for _ in range(2)]
        B_h3T = [Buf("h3T%d" % i) for i in range(2)]
        pTc = [allocR(2 * 128) for _ in range(2)]
        B_pT = [Buf("pTc%d" % i) for i in range(2)]
        sgm = [allocF(1024) for _ in range(2)]
        B_sgm = [Buf("sgm%d" % i) for i in range(2)]
        t1 = [allocF(1024) for _ in range(2)]
        B_t1 = [Buf("t1_%d" % i) for i in range(2)]
        st7 = allocF(64)
        B_st7 = [Buf("st7_%d" % i) for i in range(16)]
        P.op("pool", lambda e: e.memset(st7, 0.0), w=B_st7)
        pT_d3 = pT_d.rearrange("(c p) t -> p c t", p=128)
        def s7_front(c):
            s2 = c % 2
            pT3 = pTc[s2].rearrange("p (c n) -> p c n", c=2)
            P.op("sync", lambda e, c=c, s2=s2: e.dma_start(out=x2c[c % 3], in_=acc_d[ts(c, 128), :]), r=[B_acc], w=[B_x2[c % 3]], dma=True, sb=B_x2[c % 3])
            P.op("sync", lambda e, c=c, pT3=pT3: e.dma_start(out=pT3.bitcast(F32R), in_=pT_d3[:, :, ts(c, 128)]), w=[B_pT[s2]], dma=True, sb=B_pT[s2])
            P.op("act", lambda e, c=c, s2=s2: e.activation(out=junkA, in_=x2c[c % 3], func=AF.Square, accum_out=st7[:, c:c + 1]), r=[B_x2[c % 3]], w=[B_junkA, B_st7[c]])
            rstd_from_ss(st7[:, c:c + 1], st7[:, 16 + c:17 + c], [B_st7[c]], [B_st7[c]], D)
            P.op("dve", lambda e, c=c, s2=s2: e.scalar_tensor_tensor(out=xs3[s2], in0=x2c[c % 3], scalar=st7[:, 16 + c:17 + c], in1=gple_rep, op0=ALU.mult, op1=ALU.mult),
                 r=[B_x2[c % 3], B_st7[c], B_gpr], w=[B_xs3[s2]])
            pA, pB = 0, 1
            for dc in range(8):
                pi = pA if dc < 4 else pB
                P.op("pe", lambda e, pi=pi, dc=dc, s2=s2: e.transpose(out=psb[pi][:, ts(dc % 4, 128)], in_=xs3[s2][:, ts(dc, 128)], identity=ident),
                     r=[B_xs3[s2], B_const], w=[PS[pi]])
            evac_copy(h3T[s2][:, 0:512], psb[pA][:, :], [PS[pA]], [B_h3T[s2]])
            evac_copy(h3T[s2][:, 512:1024], psb[pB][:, :], [PS[pB]], [B_h3T[s2]])

        def s7_back(c):
            s2 = c % 2
            h3T3 = h3T[s2].rearrange("p (c n) -> p c n", c=8)
            pT3 = pTc[s2].rearrange("p (c n) -> p c n", c=2)
            pG = [2 + (c % 2) * 2, 3 + (c % 2) * 2]
            pE = [6, 7]
            for half in range(2):
                for kc in range(2):
                    P.op("pe", lambda e, half=half, kc=kc, pT3=pT3, pE=pE: e.matmul(psb[pE[half]][:, :], lhsT=pT3[:, kc, :].bitcast(F32R), rhs=wpp3[:, kc, ts(half, 512)].bitcast(F32R),
                                                                              start=(kc == 0), stop=(kc == 1)),
                         r=[B_pT[s2], B_wpp], w=[PS[pE[half]]])
            for half in range(2):
                P.op("act", lambda e, half=half, c=c, pE=pE: e.activation(out=junkA[:, 0:512], in_=psb[pE[half]][:, :], func=AF.Square, accum_out=st7[:, 32 + 16 * half + c:33 + 16 * half + c]),
                     r=[PS[pE[half]]], w=[B_junkA, B_st7[c]])
            for half in range(2):
                for dc in range(8):
                    P.op("pe", lambda e, half=half, dc=dc, h3T3=h3T3, pG=pG: e.matmul(psb[pG[half]][:, :], lhsT=h3T3[:, dc, :].bitcast(F32R), rhs=wpg3[:, dc, ts(half, 512)].bitcast(F32R),
                                                                                start=(dc == 0), stop=(dc == 7)),
                         r=[B_h3T[s2], B_wpg], w=[PS[pG[half]]])
            P.op("dve", lambda e, c=c: e.tensor_tensor(out=st7[:, 32 + c:33 + c], in0=st7[:, 32 + c:33 + c], in1=st7[:, 48 + c:49 + c], op=ALU.add), r=[B_st7[c]], w=[B_st7[c]])
            rstd_from_ss(st7[:, 32 + c:33 + c], st7[:, 48 + c:49 + c], [B_st7[c]], [B_st7[c]], D)
            for half in range(2):
                P.op("dve", lambda e, half=half, s2=s2, c=c, pE=pE: e.scalar_tensor_tensor(out=t1[s2][:, ts(half, 512)], in0=psb[pE[half]][:, :], scalar=st7[:, 48 + c:49 + c],
                                                                                     in1=gpost[:, ts(half, 512)], op0=ALU.mult, op1=ALU.mult),
                     r=[PS[pE[half]], B_st7[c], B_gpost], w=[B_t1[s2]])
            for half in range(2):
                P.op("act", lambda e, half=half, s2=s2, pG=pG: e.activation(out=sgm[s2][:, ts(half, 512)], in_=psb[pG[half]][:, :], func=AF.Sigmoid), r=[PS[pG[half]]], w=[B_sgm[s2]])
            P.op("dve", lambda e, s2=s2: e.tensor_tensor(out=t1[s2], in0=t1[s2], in1=sgm[s2], op=ALU.mult), r=[B_sgm[s2], B_t1[s2]], w=[B_t1[s2]])
            P.op("dve", lambda e, s2=s2: e.tensor_tensor(out=t1[s2], in0=t1[s2], in1=x2c[c % 3], op=ALU.add), r=[B_x2[c % 3], B_t1[s2]], w=[B_t1[s2]])
            P.op("pool", lambda e, c=c, s2=s2: e.dma_start(out=out_d[ts(c, 128), :], in_=t1[s2]), r=[B_t1[s2]], w=[B_out], dma=True, sb=B_t1[s2])

        s7_front(0)
        for c in range(NT):
            if c + 1 < NT:
                s7_front(c + 1)
            s7_back(c)
        finish()
    return nc


_NC_CACHE = {}


def _bias_tiles(rpb):
    H = rpb.shape[0]
    tiles = np.full((H, 20, 128, 512), NEG, dtype=np.float32)
    a = np.arange(128) // 64
    kc = np.arange(128) % 64
    b = np.arange(512) // 64
    qc = np.arange(512) % 64
    cs = np.clip(qc - 8, 0, 48)
    for qb, chunks in ((0, QB_CHUNKS[0]), (1, QB_CHUNKS[1]), (3, QB_CHUNKS[3])):
        r = 8 * qb + b
        rs = np.clip(r - 4, 0, 24)
        for j in chunks:
            kr = 2 * j + a
            vr = (kr[:, None] >= rs[None, :]) & (kr[:, None] <= rs[None, :] + 7)
            vc = (kc[:, None] >= cs[None, :]) & (kc[:, None] <= cs[None, :] + 15)
            valid = vr & vc
            dr = np.clip(kr[:, None] - r[None, :] + 7, 0, 14)
            dc = np.clip(kc[:, None] - qc[None, :] + 15, 0, 30)
            g = rpb[:, dr, dc]
            tiles[:, bias_tile_id(qb, j)] = np.where(valid[None], g, np.float32(NEG))
    return tiles


def kernel(x, p, norm_mix, w_in, w_pool, pool_scale, q_norm, k_norm, rpb, w_out, norm_ffn, w_router,
           w_gate, w_up, w_down, norm_ple, w_ple_gate, w_ple_proj, norm_ple_post):
    f = lambda a: np.ascontiguousarray(np.asarray(a, dtype=np.float32))
    x = f(x); p = f(p)
    B = x.shape[0]
    if "nc" not in _NC_CACHE:
        _NC_CACHE["nc"] = build_nc()
    nc = _NC_CACHE["nc"]
    col = lambda v: f(v).reshape(-1, 128).T
    gcols = np.zeros((128, 32), np.float32)
    gcols[:, 0:8] = col(norm_mix[0])
    gcols[:, 8:16] = col(norm_ffn[0])
    gcols[:, 16:24] = col(norm_ple[0])
    gcols[:, 24:28] = col(pool_scale[0])
    gcols[:, 28] = np.tile(f(q_norm[0]), 2)
    gcols[:, 29] = np.tile(f(k_norm[0]), 2)
    gpost = np.ascontiguousarray(np.broadcast_to(f(norm_ple_post[0])[None, :], (128, D)))
    bias_tiles = _bias_tiles(f(rpb[0]))
    grep = np.ascontiguousarray(np.stack([np.broadcast_to(f(norm_ffn[0])[None, :], (128, D)), np.broadcast_to(f(norm_ple[0])[None, :], (128, D))], axis=0))
    shared = dict(w_in=f(w_in[0]), w_pool=f(w_pool[0]), w_out=f(w_out[0]), w_router=f(w_router[0]),
                  w_gate=f(w_gate[0]), w_up=f(w_up[0]), w_down=f(w_down[0]), w_ple_gate=f(w_ple_gate[0]),
                  w_ple_proj=f(w_ple_proj[0]), gcols=gcols, gpost=gpost, bias_tiles=bias_tiles, grep=grep)
    in_maps = []
    for b in range(B):
        m = dict(shared)
        m["x"] = x[b]
        m["pT"] = np.ascontiguousarray(p[0, b].T)
        in_maps.append(m)
    res = run_bass_kernel_spmd(nc, in_maps, core_ids=list(range(B)))
    return np.stack([np.asarray(r["out"], dtype=np.float32) for r in res.results], axis=0)
```
